# Optimizing a Trainium2 kernel written in Bass

```python
import math
import functools
import numpy as np
import jax
import jax.numpy as jnp
from jax import lax

D_MODEL = 1024
BATCH = 4
SEQ = 4096
DEPTH = 2

GRID_W = 64
CTX_LEN = 256
EPS = 1e-6
F32 = jnp.float32

ATT_HEADS = 4
ATT_DH = 64
ATT_VD = 2 * ATT_DH
ATT_W = ATT_HEADS * ATT_VD
Q_BLOCK = 128
ROPE_BASE = 10000.0

DN_HEADS = 4
DN_DH = 64
DN_W = DN_HEADS * DN_DH
DN_CONV = 5
DN_CHUNK = 64

POOL_WINDOWS = (2, 4, 8, 16)
POOL_GD = 64
POOL_W = len(POOL_WINDOWS) * POOL_GD

N_BRANCH = 3
MIX_W = ATT_W + DN_W + POOL_W

IN_SPLITS = (ATT_HEADS * 2 * ATT_DH, ATT_HEADS * 2 * ATT_DH, ATT_W,
             DN_W, DN_W, DN_W, DN_W, 2 * DN_HEADS, 2 * DN_HEADS,
             POOL_W, N_BRANCH * D_MODEL)
IN_W = sum(IN_SPLITS)

FFN_DENSE = ((8 * D_MODEL // 3 + 255) // 256) * 256
N_EXPERTS = 8
TOP_K = 2
FFN_EXPERT = 7 * D_MODEL // 2
N_DENSE = (DEPTH + 1) // 2
N_MOE = DEPTH // 2

kernel_name = 'hybrid_diffattn_gdn_pool_moe_dit'


def _rmsnorm(x, g):
    xf = x.astype(F32)
    y = xf * lax.rsqrt(jnp.mean(xf * xf, axis=-1, keepdims=True) + EPS)
    return (y * g.astype(F32)).astype(x.dtype)


def _modulate(x, shift, scale):
    return x * (1 + scale) + shift


def _split(x, sizes):
    return jnp.split(x, np.cumsum(sizes)[:-1].tolist(), axis=-1)


def _l2norm(x):
    return x * lax.rsqrt(jnp.sum(x * x, axis=-1, keepdims=True) + EPS)


def _conv_centred(x, w):
    k = w.shape[0]
    return lax.conv_general_dilated(
        x, w[:, None, :].astype(x.dtype), window_strides=(1,),
        padding=[(k // 2, k // 2)], dimension_numbers=('NWC', 'WIO', 'NWC'),
        feature_group_count=x.shape[-1])


def _axial_rope_tables(rows):
    row = jnp.repeat(jnp.arange(rows), GRID_W).astype(F32)
    col = jnp.tile(jnp.arange(GRID_W), rows).astype(F32)
    n_freq = ATT_DH // 4
    inv = ROPE_BASE ** (-jnp.arange(n_freq, dtype=F32) / n_freq)
    ar = row[:, None] * inv
    ac = col[:, None] * inv
    return (jnp.cos(ar), jnp.sin(ar), jnp.cos(ac), jnp.sin(ac))


def _rotate(x, cos, sin):
    x1, x2 = jnp.split(x, 2, axis=-1)
    c = cos[:, None, None, :]
    s = sin[:, None, None, :]
    return jnp.concatenate([x1 * c - x2 * s, x2 * c + x1 * s], axis=-1)


def _apply_axial_rope(x, rope):
    cr, sr, cc, sc = rope
    xf = x.astype(F32)
    half = ATT_DH // 2
    out = jnp.concatenate([_rotate(xf[..., :half], cr, sr), _rotate(xf[..., half:], cc, sc)], axis=-1)
    return out.astype(x.dtype)


def _diff_softmax_attend(q, k, v, lam):
    s = jnp.einsum('bqhmd,bkhmd->bhmqk', q, k).astype(F32) * (ATT_DH ** -0.5)
    p = jax.nn.softmax(s, axis=-1)
    a = p[:, :, 0] - lam * p[:, :, 1]
    return jnp.einsum('bhqk,bkhe->bqhe', a.astype(v.dtype), v)


def _diff_attention(q_l, k_l, v_l, q_c, k_c, v_c, lam_params, subln_g, lambda_init, rope, ctx_out):
    B, S, _ = q_l.shape
    C = q_c.shape[1]
    ql = _apply_axial_rope(q_l.reshape(B, S, ATT_HEADS, 2, ATT_DH), rope)
    kl = _apply_axial_rope(k_l.reshape(B, S, ATT_HEADS, 2, ATT_DH), rope)
    vl = v_l.reshape(B, S, ATT_HEADS, ATT_VD)
    kc = k_c.reshape(B, C, ATT_HEADS, 2, ATT_DH)
    vc = v_c.reshape(B, C, ATT_HEADS, ATT_VD)
    lp = lam_params.astype(F32)
    lam = jnp.exp(jnp.sum(lp[0] * lp[1])) - jnp.exp(jnp.sum(lp[2] * lp[3])) + lambda_init
    k_all = jnp.concatenate([kc, kl], axis=1)
    v_all = jnp.concatenate([vc, vl], axis=1)
    nb = S // Q_BLOCK
    qb = jnp.moveaxis(ql.reshape(B, nb, Q_BLOCK, ATT_HEADS, 2, ATT_DH), 1, 0)
    ob = lax.map(lambda blk: _diff_softmax_attend(blk, k_all, v_all, lam), qb)
    o_l = jnp.moveaxis(ob, 0, 1).reshape(B, S, ATT_HEADS, ATT_VD)
    y_l = (_rmsnorm(o_l, subln_g) * (1 - lambda_init)).reshape(B, S, ATT_W)
    y_c = None
    if ctx_out:
        qc = q_c.reshape(B, C, ATT_HEADS, 2, ATT_DH)
        o_c = _diff_softmax_attend(qc, kc, vc, lam)
        y_c = (_rmsnorm(o_c, subln_g) * (1 - lambda_init)).reshape(B, C, ATT_W)
    return y_l, y_c


def _dn_inputs(q, k, v, a, b, conv_w, a_log, dt_bias):
    B, L, _ = q.shape
    qkv = jax.nn.silu(_conv_centred(jnp.concatenate([q, k, v], axis=-1), conv_w)).astype(F32)
    q, k, v = [t.reshape(B, L, DN_HEADS, DN_DH) for t in jnp.split(qkv, 3, axis=-1)]
    q = _l2norm(q) * (DN_DH ** -0.5)
    k = _l2norm(k)
    a = a.astype(F32).reshape(B, L, 2, DN_HEADS)
    b = b.astype(F32).reshape(B, L, 2, DN_HEADS)
    g = -jnp.exp(a_log.astype(F32)) * jax.nn.softplus(a + dt_bias.astype(F32))
    beta = jax.nn.sigmoid(b)
    return q, k, v, g, beta


def _chunk_gated_delta(q, k, v, beta, g, s0, with_output):
    B, L, H, Dk = q.shape
    Dv = v.shape[-1]
    n = L // DN_CHUNK
    cn = DN_CHUNK

    def blocks(t):
        t = jnp.moveaxis(t, 2, 1)
        t = t.reshape(B, H, n, cn, *t.shape[3:])
        return jnp.moveaxis(t, 2, 0)

    q, k, v, beta, g = (blocks(t) for t in (q, k, v, beta, g))
    G = jnp.cumsum(g, axis=-1)
    incl = jnp.tril(jnp.ones((cn, cn), dtype=bool))
    strict = jnp.tril(jnp.ones((cn, cn), dtype=bool), -1)
    decay = jnp.exp(jnp.where(incl, G[..., :, None] - G[..., None, :], -jnp.inf))
    kb = k * beta[..., None]
    m = jnp.where(strict, jnp.einsum('nbhid,nbhjd->nbhij', kb, k) * decay, 0.0)
    a = m + jnp.eye(cn, dtype=m.dtype)
    rhs = jnp.concatenate([v * beta[..., None], kb * jnp.exp(G)[..., None]], axis=-1)
    sol = lax.linalg.triangular_solve(a, rhs, left_side=True, lower=True, unit_diagonal=True)
    u, w = sol[..., :Dv], sol[..., Dv:]
    kt = k * jnp.exp(G[..., -1:] - G)[..., None]
    g_last = jnp.exp(G[..., -1])

    if with_output:
        qk = jnp.einsum('nbhid,nbhjd->nbhij', q, k) * decay
        qg = q * jnp.exp(G)[..., None]

        def step(s, xs):
            qg_i, qk_i, kt_i, u_i, w_i, gl_i = xs
            v_new = u_i - jnp.einsum('bhcd,bhde->bhce', w_i, s)
            o_i = jnp.einsum('bhcd,bhde->bhce', qg_i, s) + jnp.einsum('bhij,bhje->bhie', qk_i, v_new)
            s = s * gl_i[..., None, None] + jnp.einsum('bhcd,bhce->bhde', kt_i, v_new)
            return s, o_i

        s_fin, o = lax.scan(step, s0, (qg, qk, kt, u, w, g_last))
        o = jnp.moveaxis(o, 0, 2).reshape(B, H, L, Dv)
        return jnp.moveaxis(o, 1, 2), s_fin

    def step_state(s, xs):
        kt_i, u_i, w_i, gl_i = xs
        v_new = u_i - jnp.einsum('bhcd,bhde->bhce', w_i, s)
        return s * gl_i[..., None, None] + jnp.einsum('bhcd,bhce->bhde', kt_i, v_new), None

    s_fin, _ = lax.scan(step_state, s0, (kt, u, w, g_last))
    return None, s_fin


def _gated_deltanet(lat_parts, ctx_parts, conv_w, a_log, dt_bias, norm_g, ctx_out):
    ql, kl, vl, zl, al, bl = lat_parts
    qc, kc, vc, zc, ac, bc = ctx_parts
    lat = _dn_inputs(ql, kl, vl, al, bl, conv_w, a_log, dt_bias)
    ctx = _dn_inputs(qc, kc, vc, ac, bc, conv_w, a_log, dt_bias)
    B = ql.shape[0]
    s0 = jnp.zeros((B, DN_HEADS, DN_DH, DN_DH), F32)
    outs_l, outs_c = [], []
    for d in range(2):
        def orient(t):
            return jnp.flip(t, axis=1) if d == 1 else t
        c_in = [orient(t) for t in ctx[:3]] + [orient(ctx[4][:, :, d]), orient(ctx[3][:, :, d])]
        o_c, s_c = _chunk_gated_delta(*c_in, s0, ctx_out)
        l_in = [orient(t) for t in lat[:3]] + [orient(lat[4][:, :, d]), orient(lat[3][:, :, d])]
        o_l, _ = _chunk_gated_delta(*l_in, s_c, True)
        outs_l.append(orient(o_l))
        if ctx_out:
            outs_c.append(orient(o_c))

    def out_gate(o, z):
        Bz, L, _ = z.shape
        y = _rmsnorm(o, norm_g) * jax.nn.silu(z.astype(F32).reshape(Bz, L, DN_HEADS, DN_DH))
        return y.reshape(Bz, L, DN_W).astype(z.dtype)

    y_l = out_gate(outs_l[0] + outs_l[1], zl)
    y_c = out_gate(outs_c[0] + outs_c[1], zc) if ctx_out else None
    return y_l, y_c


def _multiscale_pool(u, pool_w, pool_scale):
    B, L, _ = u.shape
    uf = u.astype(F32)
    cs = jnp.concatenate([jnp.zeros((B, 1, POOL_W), F32), jnp.cumsum(uf, axis=1)], axis=1)
    t = jnp.arange(L)
    groups = []
    for gi, win in enumerate(POOL_WINDOWS):
        lo = jnp.clip(t - win // 2, 0, L)
        hi = jnp.clip(t - win // 2 + win, 0, L)
        sl = slice(gi * POOL_GD, (gi + 1) * POOL_GD)
        cs_g = cs[..., sl]
        mean = (cs_g[:, hi] - cs_g[:, lo]) / (hi - lo).astype(F32)[None, :, None]
        groups.append(mean - uf[..., sl])
    m = jnp.stack(groups, axis=2)
    y = jnp.einsum('blgi,gio->blgo', m, pool_w.astype(F32)).reshape(B, L, POOL_W)
    return (y * pool_scale.astype(F32)).astype(u.dtype)


def _token_mixer(u_lat, u_ctx, w_in, attn_lambda, attn_subln_g, lambda_init, dn_conv_w, dn_a_log,
                 dn_dt_bias, dn_norm_g, pool_w, pool_scale, w_branch, w_out, rope, ctx_out):
    pl = _split(u_lat @ w_in, IN_SPLITS)
    pc = _split(u_ctx @ w_in, IN_SPLITS)
    att_l, att_c = _diff_attention(pl[0], pl[1], pl[2], pc[0], pc[1], pc[2], attn_lambda,
                                   attn_subln_g, lambda_init, rope, ctx_out)
    dn_l, dn_c = _gated_deltanet(pl[3:9], pc[3:9], dn_conv_w, dn_a_log, dn_dt_bias, dn_norm_g, ctx_out)
    wb_att = w_branch[:ATT_W]
    wb_dn = w_branch[ATT_W:ATT_W + DN_W]
    wb_pool = w_branch[ATT_W + DN_W:]

    def merge(att, dn, pool, gates):
        ga, gd, gp = jnp.split(jax.nn.sigmoid(gates), N_BRANCH, axis=-1)
        return (ga * (att @ wb_att) + gd * (dn @ wb_dn) + gp * (pool @ wb_pool)) @ w_out

    y_lat = merge(att_l, dn_l, _multiscale_pool(pl[9], pool_w, pool_scale), pl[10])
    y_ctx = None
    if ctx_out:
        y_ctx = merge(att_c, dn_c, _multiscale_pool(pc[9], pool_w, pool_scale), pc[10])
    return y_lat, y_ctx


def _swiglu(u, w1, w3, w2):
    return (jax.nn.silu(u @ w1) * (u @ w3)) @ w2


def _moe_swiglu(u, router_w, w1, w3, w2):
    logits = (u @ router_w).astype(F32)
    top_v, top_i = lax.top_k(logits, TOP_K)
    top_w = jax.nn.softmax(top_v, axis=-1)
    comb = jnp.sum(jax.nn.one_hot(top_i, N_EXPERTS, dtype=F32) * top_w[..., None], axis=-2)
    out = jnp.zeros(u.shape, F32)
    for e in range(N_EXPERTS):
        out = out + comb[..., e:e + 1] * _swiglu(u, w1[e], w3[e], w2[e]).astype(F32)
    return out.astype(u.dtype)


def setup_inputs(seed: int = 0) -> dict:
    key = jax.random.key(seed)
    ks = iter(jax.random.split(key, 32))
    D = D_MODEL

    def nrm(shape, scale):
        return jax.random.normal(next(ks), shape, F32) * scale

    x = nrm((BATCH, SEQ, D), 1.0)
    c = nrm((BATCH, D), 1.0)
    ctx = nrm((BATCH, CTX_LEN, D), 1.0)
    c_ctx = nrm((D,), 1.0)
    ada_w = nrm((DEPTH, D, 6 * D), 0.5 * D ** -0.5)
    ada_b = nrm((DEPTH, 6 * D), 0.01)
    norm1_g = 1.0 + nrm((DEPTH, D), 0.05)
    norm2_g = 1.0 + nrm((DEPTH, D), 0.05)
    w_in = nrm((DEPTH, D, IN_W), D ** -0.5)
    attn_lambda = nrm((DEPTH, 4, ATT_DH), 0.1)
    attn_subln_g = 1.0 + nrm((DEPTH, ATT_VD), 0.05)
    dn_conv_w = nrm((DEPTH, DN_CONV, 3 * DN_W), DN_CONV ** -0.5)
    dn_a_log = jnp.log(jax.random.uniform(next(ks), (DEPTH, 2, DN_HEADS), F32, minval=1.0, maxval=16.0))
    dt = jnp.exp(jax.random.uniform(next(ks), (DEPTH, 2, DN_HEADS), F32,
                                    minval=math.log(1e-3), maxval=math.log(1e-1)))
    dn_dt_bias = dt + jnp.log(-jnp.expm1(-dt))
    dn_norm_g = 1.0 + nrm((DEPTH, DN_DH), 0.05)
    pool_w = nrm((DEPTH, len(POOL_WINDOWS), POOL_GD, POOL_GD), POOL_GD ** -0.5)
    pool_scale = 1.0 + nrm((DEPTH, POOL_W), 0.05)
    w_branch = jnp.concatenate([nrm((DEPTH, ATT_W, D), ATT_W ** -0.5),
                                nrm((DEPTH, DN_W, D), DN_W ** -0.5),
                                nrm((DEPTH, POOL_W, D), POOL_W ** -0.5)], axis=1)
    w_out = nrm((DEPTH, D, D), D ** -0.5)
    ffn_w1 = nrm((N_DENSE, D, FFN_DENSE), D ** -0.5)
    ffn_w3 = nrm((N_DENSE, D, FFN_DENSE), D ** -0.5)
    ffn_w2 = nrm((N_DENSE, FFN_DENSE, D), FFN_DENSE ** -0.5)
    router_w = nrm((N_MOE, D, N_EXPERTS), D ** -0.5)
    moe_w1 = nrm((N_MOE, N_EXPERTS, D, FFN_EXPERT), D ** -0.5)
    moe_w3 = nrm((N_MOE, N_EXPERTS, D, FFN_EXPERT), D ** -0.5)
    moe_w2 = nrm((N_MOE, N_EXPERTS, FFN_EXPERT, D), FFN_EXPERT ** -0.5)
    final_norm_g = 1.0 + nrm((D,), 0.05)
    return {'x': x, 'c': c, 'ctx': ctx, 'c_ctx': c_ctx, 'ada_w': ada_w, 'ada_b': ada_b,
            'norm1_g': norm1_g, 'norm2_g': norm2_g, 'w_in': w_in, 'attn_lambda': attn_lambda,
            'attn_subln_g': attn_subln_g, 'dn_conv_w': dn_conv_w, 'dn_a_log': dn_a_log,
            'dn_dt_bias': dn_dt_bias, 'dn_norm_g': dn_norm_g, 'pool_w': pool_w, 'pool_scale': pool_scale,
            'w_branch': w_branch, 'w_out': w_out, 'ffn_w1': ffn_w1, 'ffn_w3': ffn_w3, 'ffn_w2': ffn_w2,
            'router_w': router_w, 'moe_w1': moe_w1, 'moe_w3': moe_w3, 'moe_w2': moe_w2,
            'final_norm_g': final_norm_g}


def reference(x, c, ctx, c_ctx, ada_w, ada_b, norm1_g, norm2_g, w_in, attn_lambda, attn_subln_g,
              dn_conv_w, dn_a_log, dn_dt_bias, dn_norm_g, pool_w, pool_scale, w_branch, w_out,
              ffn_w1, ffn_w3, ffn_w2, router_w, moe_w1, moe_w3, moe_w2, final_norm_g):
    rows = x.shape[1] // GRID_W
    rope = _axial_rope_tables(rows)
    h_lat, h_ctx = x, ctx
    for l in range(DEPTH):
        ctx_out = l < DEPTH - 1
        lambda_init = 0.8 - 0.6 * math.exp(-0.3 * l)
        mod_lat = [m[:, None, :] for m in jnp.split(jax.nn.silu(c) @ ada_w[l] + ada_b[l], 6, axis=-1)]
        mod_ctx = jnp.split(jax.nn.silu(c_ctx) @ ada_w[l] + ada_b[l], 6, axis=-1)
        u_lat = _modulate(_rmsnorm(h_lat, norm1_g[l]), mod_lat[0], mod_lat[1])
        u_ctx = _modulate(_rmsnorm(h_ctx, norm1_g[l]), mod_ctx[0], mod_ctx[1])
        y_lat, y_ctx = _token_mixer(u_lat, u_ctx, w_in[l], attn_lambda[l], attn_subln_g[l], lambda_init,
                                    dn_conv_w[l], dn_a_log[l], dn_dt_bias[l], dn_norm_g[l], pool_w[l],
                                    pool_scale[l], w_branch[l], w_out[l], rope, ctx_out)
        h_lat = h_lat + mod_lat[2] * y_lat
        if ctx_out:
            h_ctx = h_ctx + mod_ctx[2] * y_ctx
        if l % 2 == 0:
            ffn = functools.partial(_swiglu, w1=ffn_w1[l // 2], w3=ffn_w3[l // 2], w2=ffn_w2[l // 2])
        else:
            ffn = functools.partial(_moe_swiglu, router_w=router_w[l // 2], w1=moe_w1[l // 2],
                                    w3=moe_w3[l // 2], w2=moe_w2[l // 2])
        h_lat = h_lat + mod_lat[5] * ffn(_modulate(_rmsnorm(h_lat, norm2_g[l]), mod_lat[3], mod_lat[4]))
        if ctx_out:
            h_ctx = h_ctx + mod_ctx[5] * ffn(_modulate(_rmsnorm(h_ctx, norm2_g[l]), mod_ctx[3], mod_ctx[4]))
    return _rmsnorm(h_lat, final_norm_g)
```

```python
import math
import numpy as np
from contextlib import ExitStack
import concourse.bass as bass
import concourse.mybir as mybir
from concourse.bass_utils import run_bass_kernel_spmd

F32 = mybir.dt.float32
BF16 = mybir.dt.bfloat16
AF = mybir.ActivationFunctionType
ALU = mybir.AluOpType
AX = mybir.AxisListType
ENGS = ('pe', 'act', 'dve', 'pool', 'sp')

USE_SPLIT = True
TC = 256
TL = 4096
T = TC + TL
BLKS = [(0, 256)] + [(256 + 512 * i, 512) for i in range(8)]
EPS = 1e-6
INW = 5904
NCH = T // 64


class Buf:
    __slots__ = ('w', 'rs')

    def __init__(self):
        self.w = None
        self.rs = {}


class Ins:
    __slots__ = ('eng', 'fn', 'deps', 'sig', 'seq', 'dma', 'dsem', 'dneed', 'idx')

    def __init__(self, eng, fn, dma=False):
        self.eng = eng
        self.fn = fn
        self.deps = {}
        self.dneed = {}
        self.sig = False
        self.seq = 0
        self.dma = dma
        self.dsem = None


class Prog:
    def __init__(self, nc):
        self.nc = nc
        self.ins = {e: [] for e in ENGS}
        self.dma_cnt = {}
        self.next_dsem = 0
        self.sb_off = 16512
        self.sb_stack = []
        self.free_dsems = []
        self.offcache = {}
        self.cur_rings = []
        self.bar_e = {}
        self.bar_d = {}
        self.cur_dsems = []
        self.nalloc = 0
        self.SB_LIMIT = 229312
        self.psums = [nc.alloc_psum_tensor(f"psb{i}", [128, 512], F32) for i in range(8)]
        self.psbufs = [Buf() for _ in range(8)]
        self.psi = 0
        self.bufs = {}

    def sb(self, shape, dtype, name=None):
        esz = 2 if dtype == BF16 else 4
        n = int(np.prod(shape[1:])) * esz
        n = (n + 63) // 64 * 64
        off = self.sb_off
        assert off + n <= self.SB_LIMIT, f"SBUF overflow {off}+{n} ({name})"
        self.sb_off += n
        self.nalloc += 1
        return self.nc.alloc_sbuf_tensor_at(f"{name or 't'}_{self.nalloc}", list(shape), dtype, offset=off)

    def mark(self):
        self.sb_stack.append((self.sb_off, self.cur_dsems, self.cur_rings))
        self.cur_dsems = []
        self.cur_rings = []

    def touch(self, t, b):
        idx = tuple(slice(0, 1) for _ in t.shape)
        self.dve(lambda e: e.memset(t[idx], 0.0), w=[b])

    def release(self):
        for rg in self.cur_rings:
            for t, b in zip(rg.t, rg.b):
                if b.w is not None or b.rs:
                    self.touch(t, b)
        self.free_dsems.extend(self.cur_dsems)
        self.sb_off, self.cur_dsems, self.cur_rings = self.sb_stack.pop()
        self.barrier()

    def barrier(self):
        for e in ENGS:
            for i in reversed(self.ins[e]):
                if not i.dma:
                    self.bar_e[e] = i
                    i.sig = True
                    break
        self.bar_d = {}

    def new_dsem(self):
        if self.free_dsems:
            i = self.free_dsems.pop()
        else:
            i = self.next_dsem
            self.next_dsem += 1
            self.dma_cnt[i] = 0
        self.cur_dsems.append(i)
        return i

    def ps(self):
        k = self.psi % 8
        self.psi += 1
        return self.psums[k], self.psbufs[k]

    def B(self, key):
        b = self.bufs.get(key)
        if b is None:
            b = self.bufs[key] = Buf()
        return b

    def _add_dep(self, ins, d, raw):
        if d is None or d is ins:
            return
        if d.dma:
            ins.dneed[d.dsem] = self.dma_cnt[d.dsem]
            return
        if d.eng == ins.eng and not raw and not ins.dma:
            return
        cur = ins.deps.get(d.eng)
        if cur is None or d.idx > cur.idx:
            ins.deps[d.eng] = d
        d.sig = True

    def op(self, eng, fn, r=(), w=(), dma=False, dsem=None):
        ins = Ins(eng, fn, dma)
        ins.idx = len(self.ins[eng])
        if dma:
            ins.dsem = dsem
        for b in r:
            self._add_dep(ins, b.w, True)
        for b in w:
            self._add_dep(ins, b.w, False)
            for rr in b.rs.values():
                self._add_dep(ins, rr, False)
        for e2, d in self.bar_e.items():
            if e2 != eng or dma:
                cur = ins.deps.get(e2)
                if cur is None or d.idx > cur.idx:
                    ins.deps[e2] = d
        for k, cnt in self.bar_d.items():
            if cnt > ins.dneed.get(k, 0):
                ins.dneed[k] = cnt
        if dma:
            self.dma_cnt[dsem] += 16
        key = ('d', dsem) if dma else eng
        for b in r:
            b.rs[key] = ins
        for b in w:
            b.w = ins
            b.rs = {}
        self.ins[eng].append(ins)
        return ins

    def pe(self, fn, r=(), w=()):
        return self.op('pe', fn, r, w)

    def act(self, fn, r=(), w=()):
        return self.op('act', fn, r, w)

    def dve(self, fn, r=(), w=()):
        return self.op('dve', fn, r, w)

    def pool(self, fn, r=(), w=()):
        return self.op('pool', fn, r, w)

    def dma(self, out, in_, r=(), w=(), sem=None, q='sp'):
        return self.op(q, lambda e: e.dma_start(out=out, in_=in_), r, w, dma=True, dsem=sem)

    def dmad(self, out, in_fn, r=(), w=(), sem=None, q='sp'):
        def f(e):
            off = self.offcache.get(id(e))
            if off is None:
                off = self.offcache[id(e)] = e.snap((e.partition_id() // 4) * 2048)
            return e.dma_start(out=out, in_=in_fn(off))
        return self.op(q, f, r, w, dma=True, dsem=sem)

    def emit(self):
        nc = self.nc
        for e in ENGS:
            s = 0
            for i in self.ins[e]:
                if i.sig and not i.dma:
                    s += 1
                    i.seq = s
        with ExitStack() as st:
            esem = {e: st.enter_context(nc.semaphore(f"s_{e}")) for e in ENGS if e != 'sp'}
            dsems = [st.enter_context(nc.semaphore(f"d_{k}")) for k in range(self.next_dsem)]
            block = st.enter_context(nc.Block())
            final_d = dict(self.dma_cnt)
            final_e = {e: max([i.seq for i in self.ins[e]] + [0]) for e in ENGS}

            def run(ename, eng):
                seen_e = {e: 0 for e in ENGS}
                seen_d = {}
                for i in self.ins[ename]:
                    for pe_, d in i.deps.items():
                        if d.seq > seen_e[pe_]:
                            eng.wait_ge(esem[pe_], d.seq)
                            seen_e[pe_] = d.seq
                    for k, cnt in i.dneed.items():
                        if cnt > seen_d.get(k, 0):
                            eng.wait_ge(dsems[k], cnt)
                            seen_d[k] = cnt
                    bi = i.fn(eng)
                    if i.dma:
                        bi.then_inc(dsems[i.dsem], 16)
                    elif i.sig:
                        bi.then_inc(esem[ename], 1)
                if ename == 'sp':
                    for k, cnt in final_d.items():
                        if cnt > seen_d.get(k, 0):
                            eng.wait_ge(dsems[k], cnt)
                    for e2 in ENGS:
                        if e2 != 'sp' and final_e[e2] > seen_e[e2]:
                            eng.wait_ge(esem[e2], final_e[e2])

            @block.tensor
            def _(eng):
                run('pe', eng)

            @block.scalar
            def _(eng):
                run('act', eng)

            @block.vector
            def _(eng):
                run('dve', eng)

            @block.gpsimd
            def _(eng):
                run('pool', eng)

            @block.sync
            def _(eng):
                run('sp', eng)


class Ring:
    def __init__(self, P, shape, dtype, n, name, dma=True):
        self.t = [P.sb(shape, dtype, f"{name}{i}") for i in range(n)]
        self.b = [Buf() for _ in range(n)]
        self.s = [P.new_dsem() if dma else None for _ in range(n)]
        self.i = 0
        self.n = n
        P.cur_rings.append(self)

    def next(self):
        k = self.i % self.n
        self.i += 1
        return self.t[k], self.b[k], self.s[k]


def bc(ap, shape):
    return ap.unsqueeze(len(ap.shape)).to_broadcast(list(shape))


def build(n_layers=2, debug=()):
    nc = bass.Bass("TRN2", target_bir_lowering=False)
    P = Prog(nc)
    SPLIT = (n_layers == 2) and USE_SPLIT
    TOUT = 2048 if SPLIT else TL

    def din(name, shape, dt=F32):
        return nc.dram_tensor(name, list(shape), dt, kind="ExternalInput").ap()

    def dscr(name, shape, dt=F32, out=False):
        return nc.dram_tensor(name, list(shape), dt, kind="ExternalOutput" if (out or name in debug) else "Internal").ap()

    xT = din("xT", [8, 128, T])
    cin = din("cin", [128, 8, 2])
    ada_w = din("ada_w", [2, 8, 128, 6144])
    adab = din("adab", [2, 128, 48])
    ngs = din("ngs", [128, 5, 8])
    w_in = din("w_in", [2, 8, 128, INW])
    w_inp = din("w_inp", [2, 8, 128, 1024])
    ropeC = din("ropeC", [128, T])
    ropeS = din("ropeS", [128, T])
    lam_in = din("lam_in", [2, 256])
    sublng = din("sublng", [128, 2])
    convw = din("convw", [2, 128, 6, 5])
    dnc = din("dnc", [2, 16])
    dnng = din("dnng", [2, 256])
    pinv = din("pinv", [2, 128, 4376])
    poolw = din("poolw", [2, 2, 128, 128])
    pools = din("pools", [128, 2, 2])
    w_br = din("w_br", [2, 8, 128, 1024])
    w_out = din("w_out", [2, 8, 128, 1024])
    fw1 = din("fw1", [8, 128, 2816])
    fw3 = din("fw3", [8, 128, 2816])
    fw2 = din("fw2", [22, 128, 1024])
    rw = din("rw", [128, 8, 8])
    mw1 = din("mw1", [8, 8, 128, 3584])
    mw3 = din("mw3", [8, 8, 128, 3584])
    mw2 = din("mw2", [8, 28, 128, 1024])
    selc = din("selc", [8, 8, 128])
    masks = din("masks", [64, 6, 64])
    identb = din("identb", [128, 128])
    yT = dscr("yT", [8, 128, TOUT], out=True)

    TP = T + 64
    H = dscr("H", [8, 128, TP])
    QT = dscr("QT", [4, 128, TP], BF16)
    KT = dscr("KT", [4, 128, T], BF16)
    VT = dscr("VT", [34, 128, 512], BF16)
    DNF = dscr("DNF", [4, 128, T], BF16)
    DNT = dscr("DNT", [T, 512], BF16)
    ZAB = dscr("ZAB", [T, 272])
    POOLT = dscr("POOLT", [2, 128, TP], BF16)
    GT = dscr("GT", [24, 128, TP], BF16)
    ATT = dscr("ATT", [4, 128, T], BF16)
    PREPB = dscr("PREPB", [2, NCH, 64, 1024], BF16)
    PREPF = dscr("PREPF", [2, NCH, 64, 12])
    OSC = dscr("OSC", [2, T, 256])
    DNY = dscr("DNY", [2, 128, TP], BF16)

    ones_f = P.sb([128, 128], F32, "ones_f"); onesb = Buf()
    ones_b = P.sb([128, 128], BF16, "ones_b"); ones_bb = Buf()
    blk_f = P.sb([128, 128], F32, "blk_f"); blkb = Buf()
    epsc = P.sb([128, 1], F32, "epsc"); epsb = Buf()
    idb = P.sb([128, 128], BF16, "idb"); idbb = Buf(); ids = P.new_dsem()
    idf = P.sb([128, 128], F32, "idf"); idfb = Buf()
    ng = P.sb([128, 5, 8], F32, "ng"); ngb = Buf(); ngsem = P.new_dsem()
    csil = P.sb([128, 8, 2], F32, "csil"); csb = Buf(); cssem = P.new_dsem()
    mod = P.sb([128, 2, 48], F32, "mod"); modb = Buf()
    gain = P.sb([128, 2, 2, 8], F32, "gain"); gainb = Buf()
    adb = P.sb([128, 48], F32, "adb"); adbb = Buf(); adsem = P.new_dsem()
    P.pool(lambda e: e.memset(ones_f[:], 1.0), w=[onesb])
    P.pool(lambda e: e.memset(ones_b[:], 1.0), w=[ones_bb])
    P.pool(lambda e: e.memset(blk_f[:], 0.0), w=[blkb])
    P.pool(lambda e: e.memset(blk_f[0:64, 0:64], 1.0), w=[blkb])
    P.pool(lambda e: e.memset(blk_f[64:128, 64:128], 1.0), w=[blkb])
    P.pool(lambda e: e.memset(epsc[:], EPS), w=[epsb])
    P.dma(idb[:], identb, w=[idbb], sem=ids, q='pool')
    P.dma(idf[:], identb, w=[idfb], sem=ids)
    P.dma(ng[:], ngs, w=[ngb], sem=ngsem)
    P.dma(csil[:], cin, w=[csb], sem=cssem)
    P.act(lambda e: e.activation(out=csil[:], in_=csil[:], func=AF.Silu), r=[csb], w=[csb])

    def dbg_dump(name, ap_dram):
        pass

    def ada_layer(l):
        P.mark()
        aw = Ring(P, [128, 8, 1024], F32, 2, "aw")
        P.dma(adb[:], adab[l], w=[adbb], sem=adsem)
        ps, pb = P.ps()
        for j in range(6):
            t, b, s = aw.next()
            for k in range(8):
                P.dma(t[:, k, :], ada_w[l, k, :, j * 1024:(j + 1) * 1024], w=[b], sem=s)
            for c in range(8):
                col = (j * 8 + c) * 2
                for k in range(8):
                    P.pe(lambda e, t=t, k=k, c=c, col=col: e.matmul(ps[:, col:col + 2], lhsT=t[:, k, c * 128:(c + 1) * 128],
                                                                    rhs=csil[:, k, :], start=(k == 0), stop=(k == 7)),
                         r=[b, csb], w=[pb])
        psv = ps[:, 0:96].rearrange("p (a s) -> p a s", s=2)
        for s_ in range(2):
            P.dve(lambda e, s_=s_: e.tensor_tensor(out=mod[:, s_, :], in0=psv[:, :, s_], in1=adb[:], op=ALU.add),
                  r=[pb, adbb], w=[modb])
            P.dve(lambda e, s_=s_: e.scalar_tensor_tensor(out=gain[:, 0, s_, :], in0=mod[:, s_, 8:16], scalar=1.0,
                                                          in1=ng[:, l, :], op0=ALU.add, op1=ALU.mult),
                  r=[modb, ngb], w=[gainb])
            P.dve(lambda e, s_=s_: e.scalar_tensor_tensor(out=gain[:, 1, s_, :], in0=mod[:, s_, 32:40], scalar=1.0,
                                                          in1=ng[:, 2 + l, :], op0=ALU.add, op1=ALU.mult),
                  r=[modb, ngb], w=[gainb])
        P.release()

    def ln_block(hsrc, hb_, n, gcol, scol, out_bf, outb, t0o, rs, f32hook=None):
        sq, sqb, _ = rs['sq'].next()
        P.act(lambda e: e.activation(out=sq[:, :, :n], in_=hsrc[:, :, :n], func=AF.Square), r=[hb_], w=[sqb])
        ps, pb = P.ps()
        for c in range(8):
            P.pe(lambda e, c=c: e.matmul(ps[:, :n], lhsT=ones_f[:], rhs=sq[:, c, :n], start=(c == 0), stop=(c == 7)),
                 r=[onesb, sqb], w=[pb])
        rstd, rb, _ = rs['rstd'].next()
        P.act(lambda e: e.activation(out=rstd[:, :n], in_=ps[:, :n], func=AF.Sqrt, bias=epsc[:], scale=1.0 / 1024),
              r=[pb, epsb], w=[rb])
        P.dve(lambda e: e.reciprocal(out=rstd[:, :n], in_=rstd[:, :n]), r=[rb], w=[rb])
        for c in range(8):
            tmp, tb, tsem = rs['tmp'].next()
            P.dve(lambda e, c=c, tmp=tmp: e.scalar_tensor_tensor(out=tmp[:, :n], in0=hsrc[:, c, :n], scalar=gcol(c),
                                                                 in1=rstd[:, :n], op0=ALU.mult, op1=ALU.mult),
                  r=[hb_, gainb, rb, ngb], w=[tb])
            if scol is None:
                f32hook(c, tmp, tb, tsem)
                continue
            if f32hook is None:
                P.act(lambda e, c=c, tmp=tmp: e.activation(out=out_bf[:, c, t0o:t0o + n], in_=tmp[:, :n], func=AF.Identity,
                                                           bias=scol(c), scale=1.0), r=[tb, modb], w=[outb])
            else:
                P.act(lambda e, c=c, tmp=tmp: e.activation(out=tmp[:, :n], in_=tmp[:, :n], func=AF.Identity,
                                                           bias=scol(c), scale=1.0), r=[tb, modb], w=[tb])
                P.pool(lambda e, c=c, tmp=tmp: e.tensor_copy(out=out_bf[:, c, t0o:t0o + n], in_=tmp[:, :n]), r=[tb], w=[outb])
                f32hook(c, tmp, tb, tsem)

    def ln_rings():
        return {'sq': Ring(P, [128, 8, 512], F32, 1, "sq"), 'rstd': Ring(P, [128, 512], F32, 2, "rstd"),
                'tmp': Ring(P, [128, 512], F32, 3, "tmp")}

    def hsrc_of(l):
        return xT if l == 0 else H

    def phase_proj(l):
        ctx_out = l == 0
        P.mark()
        uT = P.sb([128, 8, T], BF16, "uT"); ub = Buf()
        P.mark()
        rs = ln_rings()
        hr = Ring(P, [128, 8, 512], F32, 2, "hblk")
        src = hsrc_of(l)
        for (t0, n) in BLKS:
            s_ = 1 if t0 == 0 else 0
            h, hb_, hs = hr.next()
            P.dma(h[:, :, :n], src[:, :, t0:t0 + n].rearrange("c p t -> p c t"), r=[P.B(('H', t0))], w=[hb_], sem=hs)
            ln_block(h, hb_, n, lambda c, s_=s_: gain[:, 0, s_, c:c + 1], lambda c, s_=s_: mod[:, s_, c:c + 1],
                     uT, ub, t0, rs)
        P.release()

        wrs = {512: Ring(P, [128, 8, 512], BF16, 2, "wt512"), 272: Ring(P, [128, 8, 272], BF16, 1, "wt272"),
               128: Ring(P, [128, 8, 128], BF16, 4, "wt128")}
        wl = w_in[l]

        def loadw(c0, w, src_=None):
            t, b, s = wrs[w].next()
            P.dma(t[:], (src_ if src_ is not None else wl)[:, :, c0:c0 + w].rearrange("c p n -> p c n"),
                  w=[b], sem=s, q='pool')
            return t, b

        def gemm_fm(wt, wb_, j0, t0, n):
            ps, pb = P.ps()
            for c in range(8):
                P.pe(lambda e, c=c: e.matmul(ps[:, :n], lhsT=wt[:, c, j0:j0 + 128], rhs=uT[:, c, t0:t0 + n],
                                             start=(c == 0), stop=(c == 7)), r=[wb_, ub], w=[pb])
            return ps, pb

        P.mark()
        rc = P.sb([128, T], F32, "ropeC"); rcb = Buf(); rsm = P.new_dsem()
        rsn = P.sb([128, T], F32, "ropeS"); rsb = Buf()
        P.dma(rc[:], ropeC, w=[rcb], sem=rsm)
        P.dma(rsn[:], ropeS, w=[rsb], sem=rsm)
        t1r = Ring(P, [128, 512], F32, 2, "t1")
        t2r = Ring(P, [128, 512], F32, 2, "t2")
        obr = Ring(P, [128, T], BF16, 2, "qkout")
        for nm, col0, dst in (('k', 512, KT), ('q', 0, QT)):
            for hh in range(4):
                wt, wb_ = loadw(col0 + hh * 128, 128)
                wt2, wb2 = loadw(col0 + hh * 128, 128, w_inp[l])
                ob, obb, obs = obr.next()
                for (t0, n) in BLKS:
                    if nm == 'q' and t0 == 0 and not ctx_out:
                        continue
                    ps1, pb1 = gemm_fm(wt, wb_, 0, t0, n)
                    ps2, pb2 = gemm_fm(wt2, wb2, 0, t0, n)
                    t1, t1b, _ = t1r.next()
                    t2, t2b, _ = t2r.next()
                    P.dve(lambda e, ps1=ps1, t1=t1, t0=t0, n=n: e.tensor_tensor(out=t1[:, :n], in0=ps1[:, :n], in1=rc[:, t0:t0 + n], op=ALU.mult),
                          r=[pb1, rcb], w=[t1b])
                    P.dve(lambda e, ps2=ps2, t2=t2, t0=t0, n=n: e.tensor_tensor(out=t2[:, :n], in0=ps2[:, :n], in1=rsn[:, t0:t0 + n], op=ALU.mult),
                          r=[pb2, rsb], w=[t2b])
                    P.pool(lambda e, t1=t1, t2=t2, ob=ob, t0=t0, n=n: e.tensor_tensor(out=ob[:, t0:t0 + n], in0=t1[:, :n], in1=t2[:, :n], op=ALU.add),
                           r=[t1b, t2b], w=[obb])
                lo = 0 if (nm == 'k' or ctx_out) else 256
                P.dma(dst[hh, :, lo:T], ob[:, lo:T], r=[obb], w=[P.B((nm, hh))], sem=obs)
        P.release()

        P.mark()
        vst = Ring(P, [128, 512], BF16, 3, "vst")
        wt, wb_ = loadw(1024, 512)
        for tt in range(34):
            ps, pb = P.ps()
            for c in range(8):
                P.pe(lambda e, c=c, ps=ps, tt=tt, wt=wt: e.matmul(ps[:, :], lhsT=uT[:, c, tt * 128:(tt + 1) * 128], rhs=wt[:, c, :512],
                                                           start=(c == 0), stop=(c == 7)), r=[wb_, ub], w=[pb])
            st, sb_, ss = vst.next()
            P.act(lambda e, ps=ps, st=st: e.copy(out=st[:], in_=ps[:]), r=[pb], w=[sb_])
            P.dma(VT[tt], st[:], r=[sb_], w=[P.B(('V', tt))], sem=ss)
        zst = Ring(P, [128, 272], F32, 3, "zst")
        dcn = P.sb([128, 16], F32, "dcn"); dcb = Buf(); dcs = P.new_dsem()
        P.dma(dcn[:], dnc[l].partition_broadcast(128), w=[dcb], sem=dcs)
        P.act(lambda e: e.activation(out=dcn[:, 8:16], in_=dcn[:, 8:16], func=AF.Exp), r=[dcb], w=[dcb])
        P.dve(lambda e: e.tensor_scalar(out=dcn[:, 8:16], in0=dcn[:, 8:16], scalar1=-1.0, scalar2=None, op0=ALU.mult), r=[dcb], w=[dcb])
        wt, wb_ = loadw(2304, 272)
        for tt in range(34):
            ps, pb = P.ps()
            for c in range(8):
                P.pe(lambda e, c=c, ps=ps, tt=tt, wt=wt: e.matmul(ps[:, :272], lhsT=uT[:, c, tt * 128:(tt + 1) * 128], rhs=wt[:, c, :272],
                                                           start=(c == 0), stop=(c == 7)), r=[wb_, ub], w=[pb])
            st, sb_, ss = zst.next()
            P.act(lambda e, ps=ps, st=st: e.activation(out=st[:, 0:256], in_=ps[:, 0:256], func=AF.Silu), r=[pb], w=[sb_])
            P.dve(lambda e, ps=ps, st=st: e.tensor_tensor(out=st[:, 256:264], in0=ps[:, 256:264], in1=dcn[:, 0:8], op=ALU.add), r=[pb, dcb], w=[sb_])
            P.act(lambda e, st=st: e.activation(out=st[:, 256:264], in_=st[:, 256:264], func=AF.Exp), r=[sb_], w=[sb_])
            P.act(lambda e, st=st: e.activation(out=st[:, 256:264], in_=st[:, 256:264], func=AF.Ln, bias=1.0, scale=1.0), r=[sb_], w=[sb_])
            P.dve(lambda e, st=st: e.tensor_tensor(out=st[:, 256:264], in0=st[:, 256:264], in1=dcn[:, 8:16], op=ALU.mult), r=[sb_, dcb], w=[sb_])
            P.act(lambda e, ps=ps, st=st: e.activation(out=st[:, 264:272], in_=ps[:, 264:272], func=AF.Sigmoid), r=[pb], w=[sb_])
            P.dma(ZAB[tt * 128:(tt + 1) * 128, :], st[:], r=[sb_], w=[P.B(('Z', tt))], sem=ss)
        P.release()

        P.mark()
        gst = Ring(P, [128, 512], BF16, 4, "gst")
        for g4 in range(6):
            wt, wb_ = loadw(2832 + g4 * 512, 512)
            for j in range(4):
                for (t0, n) in BLKS:
                    if t0 == 0 and not ctx_out:
                        continue
                    ps, pb = gemm_fm(wt, wb_, j * 128, t0, n)
                    st, sb_, ss = gst.next()
                    P.act(lambda e, ps=ps, st=st, n=n: e.activation(out=st[:, :n], in_=ps[:, :n], func=AF.Sigmoid), r=[pb], w=[sb_])
                    P.dma(GT[g4 * 4 + j, :, t0:t0 + n], st[:, :n], r=[sb_], w=[P.B(('G', g4 * 4 + j, t0))], sem=ss)
        P.release()

        P.mark()
        raw = P.sb([128, 4358], F32, "raw"); rawb = Buf()
        acc = P.sb([128, 4358], F32, "acc"); accb = Buf()
        sq2 = P.sb([128, 4358], F32, "sq2"); sq2b = Buf()
        nrm = Ring(P, [128, 4358], BF16, 2, "nrm")
        cw = P.sb([128, 6, 5], F32, "cw"); cwb = Buf(); cws = P.new_dsem()
        rsr = Ring(P, [128, 512], F32, 2, "rs2")
        tst = Ring(P, [128, 128], BF16, 3, "tst")
        P.dma(cw[:], convw[l], w=[cwb], sem=cws)
        P.pool(lambda e: e.memset(raw[:], 0.0), w=[rawb])
        NW = 4354
        for i in range(6):
            wt, wb_ = loadw(1536 + i * 128, 128)
            for (t0, n) in BLKS:
                o = t0 + 2 if t0 == 0 else t0 + 4
                ps, pb = gemm_fm(wt, wb_, 0, t0, n)
                P.act(lambda e, ps=ps, o=o, n=n: e.copy(out=raw[:, o:o + n], in_=ps[:, :n]), r=[pb], w=[rawb])
            P.dve(lambda e, i=i: e.tensor_scalar(out=acc[:, 2:2 + NW], in0=raw[:, 0:NW], scalar1=cw[:, i, 0:1], scalar2=None, op0=ALU.mult),
                  r=[rawb, cwb], w=[accb])
            for k in range(1, 5):
                eng = P.dve
                eng(lambda e, i=i, k=k: e.scalar_tensor_tensor(out=acc[:, 2:2 + NW], in0=raw[:, k:k + NW], scalar=cw[:, i, k:k + 1],
                                                               in1=acc[:, 2:2 + NW], op0=ALU.mult, op1=ALU.add),
                    r=[rawb, cwb, accb], w=[accb])
            P.act(lambda e: e.activation(out=acc[:, 2:2 + NW], in_=acc[:, 2:2 + NW], func=AF.Silu), r=[accb], w=[accb])
            nr, nrb, nrs = nrm.next()
            if i < 4:
                P.pool(lambda e: e.tensor_tensor(out=sq2[:, 2:2 + NW], in0=acc[:, 2:2 + NW], in1=acc[:, 2:2 + NW], op=ALU.mult),
                       r=[accb], w=[sq2b])
                for (t0, n) in BLKS:
                    o = t0 + 2 if t0 == 0 else t0 + 4
                    ps, pb = P.ps()
                    P.pe(lambda e, ps=ps, o=o, n=n: e.matmul(ps[:, :n], lhsT=blk_f[:], rhs=sq2[:, o:o + n], start=True, stop=True),
                         r=[blkb, sq2b], w=[pb])
                    r2, r2b, _ = rsr.next()
                    P.act(lambda e, ps=ps, r2=r2, n=n: e.activation(out=r2[:, :n], in_=ps[:, :n], func=AF.Sqrt, bias=epsc[:], scale=1.0),
                          r=[pb, epsb], w=[r2b])
                    P.dve(lambda e, r2=r2, n=n: e.reciprocal(out=r2[:, :n], in_=r2[:, :n]), r=[r2b], w=[r2b])
                    P.dve(lambda e, r2=r2, nr=nr, o=o, n=n, i=i: e.scalar_tensor_tensor(
                        out=nr[:, o:o + n], in0=acc[:, o:o + n], scalar=(0.125 if i < 2 else 1.0), in1=r2[:, :n],
                        op0=ALU.mult, op1=ALU.mult), r=[accb, r2b], w=[nrb])
                P.dma(DNF[i, :, 0:256], nr[:, 2:258], r=[nrb], w=[P.B(('DNF', i))], sem=nrs)
                P.dma(DNF[i, :, 256:T], nr[:, 260:4356], r=[nrb], w=[P.B(('DNF', i))], sem=nrs)
            else:
                P.pool(lambda e, nr=nr: e.tensor_copy(out=nr[:, 2:2 + NW], in_=acc[:, 2:2 + NW]), r=[accb], w=[nrb])
            if i >= 2:
                for tt in range(34):
                    o = tt * 128 + 2 if tt < 2 else tt * 128 + 4
                    ps, pb = P.ps()
                    psv = ps[:, 0:64].bitcast(BF16)
                    P.pe(lambda e, psv=psv, nr=nr, o=o: e.transpose(out=psv, in_=nr[:, o:o + 128], identity=idb[:]),
                         r=[nrb, idbb], w=[pb])
                    st, sb_, ss = tst.next()
                    P.act(lambda e, psv=psv, st=st: e.copy(out=st[:], in_=psv), r=[pb], w=[sb_])
                    P.dma(DNT[tt * 128:(tt + 1) * 128, (i - 2) * 128:(i - 1) * 128], st[:], r=[sb_], w=[P.B(('DNT', tt, i))], sem=ss)
        P.release()

        P.mark()
        praw = P.sb([128, 4376], F32, "praw"); prb = Buf()
        pa = P.sb([128, 4376], F32, "pa"); pab = Buf()
        pbt = P.sb([128, 4376], F32, "pbt"); pbb = Buf()
        pv = P.sb([128, 4376], F32, "pv"); pvb = Buf(); pvs = P.new_dsem()
        pw = P.sb([128, 128], F32, "pw"); pwb = Buf(); pws = P.new_dsem()
        psc = P.sb([128, 2, 2], F32, "psc"); pscb = Buf()
        pst = Ring(P, [128, T], BF16, 1, "pst")
        P.dma(psc[:], pools, w=[pscb], sem=pws)
        P.pool(lambda e: e.memset(praw[:], 0.0), w=[prb])
        for i in range(2):
            wt, wb_ = loadw(2576 + i * 128, 128)
            P.dma(pv[:], pinv[i], w=[pvb], sem=pvs)
            P.dma(pw[:], poolw[l, i], w=[pwb], sem=pws)
            for (t0, n) in BLKS:
                o = t0 + 8 if t0 == 0 else t0 + 16
                ps, pb = gemm_fm(wt, wb_, 0, t0, n)
                P.act(lambda e, ps=ps, o=o, n=n: e.copy(out=praw[:, o:o + n], in_=ps[:, :n]), r=[pb], w=[prb])
            P.dve(lambda e: e.tensor_tensor(out=pa[:, 1:4376], in0=praw[:, 0:4375], in1=praw[:, 1:4376], op=ALU.add), r=[prb], w=[pab])
            P.pool(lambda e: e.memset(pa[:, 0:1], 0.0), w=[pab])
            P.pool(lambda e: e.memset(pbt[:, 0:8], 0.0), w=[pbb])
            P.pool(lambda e: e.memset(pbt[:, 4368:4376], 0.0), w=[pbb])
            P.dve(lambda e: e.tensor_tensor(out=pbt[:, 1:4375], in0=pa[:, 0:4374], in1=pa[:, 2:4376], op=ALU.add), r=[pab], w=[pbb])
            if i == 0:
                lo_src, lob, hi_src, hib = pa, pab, pbt, pbb
            else:
                P.pool(lambda e: e.memset(pa[:, 0:8], 0.0), w=[pab])
                P.pool(lambda e: e.memset(pa[:, 4368:4376], 0.0), w=[pab])
                P.dve(lambda e: e.tensor_tensor(out=pa[:, 2:4374], in0=pbt[:, 0:4372], in1=pbt[:, 4:4376], op=ALU.add), r=[pbb], w=[pab])
                P.dve(lambda e: e.tensor_tensor(out=pbt[:, 4:4372], in0=pa[:, 0:4368], in1=pa[:, 8:4376], op=ALU.add), r=[pab], w=[pbb])
                lo_src, lob, hi_src, hib = pa, pab, pbt, pbb
            P.dve(lambda e, s_=lo_src: e.tensor_tensor(out=s_[0:64, 8:4368], in0=s_[0:64, 8:4368], in1=pv[0:64, 8:4368], op=ALU.mult), r=[lob, pvb], w=[lob])
            P.pool(lambda e, s_=lo_src: e.tensor_tensor(out=s_[0:64, 8:4368], in0=s_[0:64, 8:4368], in1=praw[0:64, 8:4368], op=ALU.subtract), r=[lob, prb], w=[lob])
            P.dve(lambda e, s_=hi_src: e.tensor_tensor(out=s_[64:128, 8:4368], in0=s_[64:128, 8:4368], in1=pv[64:128, 8:4368], op=ALU.mult), r=[hib, pvb], w=[hib])
            P.pool(lambda e, s_=hi_src, d_=lo_src: e.tensor_tensor(out=d_[64:128, 8:4368], in0=s_[64:128, 8:4368], in1=praw[64:128, 8:4368], op=ALU.subtract), r=[hib, prb, lob], w=[lob])
            st, sb_, ss = pst.next()
            for (t0, n) in BLKS:
                o = t0 + 8 if t0 == 0 else t0 + 16
                ps, pb = P.ps()
                P.pe(lambda e, ps=ps, o=o, n=n, s_=lo_src: e.matmul(ps[:, :n], lhsT=pw[:], rhs=s_[:, o:o + n], start=True, stop=True),
                     r=[pwb, lob], w=[pb])
                P.act(lambda e, ps=ps, st=st, t0=t0, n=n, i=i: e.activation(out=st[:, t0:t0 + n], in_=ps[:, :n], func=AF.Copy, scale=psc[:, l, i:i + 1]),
                      r=[pb, pscb], w=[sb_])
            P.dma(POOLT[i, :, 0:T], st[:], r=[sb_], w=[P.B(('POOL', i))], sem=ss)
        P.release()
        P.release()

    def phase_attn(l):
        ctx_out = l == 0
        lam_init = 0.8 - 0.6 * math.exp(-0.3 * l)
        P.mark()
        lt = P.sb([128, 4, 64], F32, "lt"); ltb = Buf(); lts = P.new_dsem()
        l2 = P.sb([128, 2, 64], F32, "l2"); l2b = Buf()
        lr = P.sb([128, 2], F32, "lr"); lrb = Buf()
        nlam = P.sb([128, 1], F32, "nlam"); nlb = Buf()
        sg = P.sb([128, 2], F32, "sg"); sgb = Buf()
        eps128 = P.sb([128, 1], F32, "e128"); e128b = Buf()
        P.dma(lt[:], lam_in[l].rearrange("(a b) -> a b", a=4).partition_broadcast(128), w=[ltb], sem=lts)
        P.dma(sg[:], sublng, w=[sgb], sem=lts)
        ltv = lt[:].rearrange("p (x y) d -> p x y d", y=2)
        P.dve(lambda e: e.tensor_tensor(out=l2[:], in0=ltv[:, :, 0, :], in1=ltv[:, :, 1, :], op=ALU.mult), r=[ltb], w=[l2b])
        P.dve(lambda e: e.tensor_reduce(out=lr[:], in_=l2[:], axis=AX.X, op=ALU.add), r=[l2b], w=[lrb])
        P.act(lambda e: e.activation(out=lr[:], in_=lr[:], func=AF.Exp), r=[lrb], w=[lrb])
        P.dve(lambda e: e.scalar_tensor_tensor(out=nlam[:], in0=lr[:, 1:2], scalar=-lam_init, in1=lr[:, 0:1], op0=ALU.add, op1=ALU.subtract),
              r=[lrb], w=[nlb])
        P.pool(lambda e: e.memset(eps128[:], EPS), w=[e128b])
        kt = P.sb([128, T], BF16, "kt"); ktb = Buf(); kts = P.new_dsem()
        qt = P.sb([128, T], BF16, "qt"); qtb = Buf(); qts = P.new_dsem()
        vt = P.sb([128, 34, 128], BF16, "vt"); vtb = Buf(); vts = P.new_dsem()
        pr = Ring(P, [128, 512], BF16, 4, "pT")
        rr = Ring(P, [128, 512], F32, 2, "rr")
        tr = Ring(P, [128, 512], F32, 3, "tr")
        ost = Ring(P, [128, 512], BF16, 2, "ost")
        lacc = [[(P.sb([128, 512], F32, f"lacc{m}{k}"), Buf()) for k in range(3)] for m in range(2)]
        own = SPLIT and l == 1

        def attn_block(hh, t0, n):
            nkt = 2 if (t0 == 0 and not own) else 34
            accs = [P.ps() for _ in range(4)]
            sbanks = [P.ps() for _ in range(4)]
            iters = [(m, ki) for m in range(2) for ki in range(nkt)]
            for m in range(2):
                for k in range(3):
                    la, lab = lacc[m][k]
                    P.pool(lambda e, la=la: e.memset(la[:], 0.0), w=[lab])
            wbk, wbkb = sbanks[3]
            for _ in range(14):
                P.pe(lambda e: e.matmul(wbk[:, :512], lhsT=ones_b[:], rhs=kt[:, 0:512], start=True, stop=True), r=[ones_bb, ktb], w=[wbkb])

            def issue_S(j):
                m, ki = iters[j]
                psS, psb_ = sbanks[j % 4]
                P.pe(lambda e: e.matmul(psS[:, :n], lhsT=kt[m * 64:(m + 1) * 64, ki * 128:(ki + 1) * 128],
                                        rhs=qt[m * 64:(m + 1) * 64, t0:t0 + n], start=True, stop=True),
                     r=[ktb, qtb], w=[psb_])

            def issue_rest(j):
                m, ki = iters[j]
                psS, psb_ = sbanks[j % 4]
                po, pob = accs[2 * m]
                pl_, plb = accs[2 * m + 1]
                pT, pTb, _ = pr.next()
                P.act(lambda e: e.activation(out=pT[:, :n], in_=psS[:, :n], func=AF.Exp, scale=0.125), r=[psb_], w=[pTb])
                P.pe(lambda e: e.matmul(po[:, :n], lhsT=vt[:, ki, :], rhs=pT[:, :n], start=(ki == 0), stop=(ki == nkt - 1)),
                     r=[vtb, pTb], w=[pob])
                if ki % 3 == 2:
                    la, lab = lacc[m][2]
                    P.pool(lambda e: e.tensor_tensor(out=la[:, :n], in0=la[:, :n], in1=pT[:, :n], op=ALU.add), r=[lab, pTb], w=[lab])
                else:
                    la, lab = lacc[m][(ki // 3 + ki % 3) % 2]
                    P.dve(lambda e: e.tensor_tensor(out=la[:, :n], in0=la[:, :n], in1=pT[:, :n], op=ALU.add), r=[lab, pTb], w=[lab])

            LOOK = 2
            for j in range(len(iters)):
                issue_S(j)
                if j >= LOOK:
                    issue_rest(j - LOOK)
            for j in range(max(0, len(iters) - LOOK), len(iters)):
                issue_rest(j)
            attn_tail(hh, t0, n, accs, sbanks)

        def attn_tail(hh, t0, n, accs, sbanks):
            (po0, pob0), (pl0, plb0), (po1, pob1), (pl1, plb1) = accs
            for m, (pl_, plb) in ((0, (pl0, plb0)), (1, (pl1, plb1))):
                (la0, la0b), (la1, la1b), (la2, la2b) = lacc[m]
                P.dve(lambda e, la0=la0, la1=la1: e.tensor_tensor(out=la0[:, :n], in0=la0[:, :n], in1=la1[:, :n], op=ALU.add), r=[la0b, la1b], w=[la0b])
                P.dve(lambda e, la0=la0, la2=la2: e.tensor_tensor(out=la0[:, :n], in0=la0[:, :n], in1=la2[:, :n], op=ALU.add), r=[la0b, la2b], w=[la0b])
                P.pe(lambda e, pl_=pl_, la0=la0: e.matmul(pl_[:, :n], lhsT=ones_f[:], rhs=la0[:, :n], start=True, stop=True), r=[onesb, la0b], w=[plb])
            r0, r0b, _ = rr.next()
            r1, r1b, _ = rr.next()
            P.dve(lambda e, r0=r0, pl0=pl0, n=n: e.reciprocal(out=r0[:, :n], in_=pl0[:, :n]), r=[plb0], w=[r0b])
            P.dve(lambda e, r1=r1, pl1=pl1, n=n: e.reciprocal(out=r1[:, :n], in_=pl1[:, :n]), r=[plb1], w=[r1b])
            ta, tab, _ = tr.next()
            tb_, tbb, _ = tr.next()
            P.dve(lambda e, ta=ta, po0=po0, r0=r0, n=n: e.tensor_tensor(out=ta[:, :n], in0=po0[:, :n], in1=r0[:, :n], op=ALU.mult), r=[pob0, r0b], w=[tab])
            P.dve(lambda e, tb_=tb_, po1=po1, r1=r1, n=n: e.tensor_tensor(out=tb_[:, :n], in0=po1[:, :n], in1=r1[:, :n], op=ALU.mult), r=[pob1, r1b], w=[tbb])
            P.dve(lambda e, ta=ta, tb_=tb_, n=n: e.scalar_tensor_tensor(out=ta[:, :n], in0=tb_[:, :n], scalar=nlam[:, 0:1], in1=ta[:, :n], op0=ALU.mult, op1=ALU.add),
                   r=[tab, tbb, nlb], w=[tab])
            P.act(lambda e, ta=ta, tb_=tb_, n=n: e.activation(out=tb_[:, :n], in_=ta[:, :n], func=AF.Square), r=[tab], w=[tbb])
            pss, pssb = sbanks[0]
            P.pe(lambda e, pss=pss, tb_=tb_, n=n: e.matmul(pss[:, :n], lhsT=ones_f[:], rhs=tb_[:, :n], start=True, stop=True), r=[onesb, tbb], w=[pssb])
            P.act(lambda e, pss=pss, tb_=tb_, n=n: e.activation(out=tb_[:, :n], in_=pss[:, :n], func=AF.Sqrt, bias=eps128[:], scale=1.0 / 128), r=[pssb, e128b], w=[tbb])
            P.dve(lambda e, tb_=tb_, n=n: e.reciprocal(out=tb_[:, :n], in_=tb_[:, :n]), r=[tbb], w=[tbb])
            st, sb_, ss = ost.next()
            P.dve(lambda e, st=st, ta=ta, tb_=tb_, n=n: e.scalar_tensor_tensor(out=st[:, :n], in0=ta[:, :n], scalar=sg[:, l:l + 1], in1=tb_[:, :n], op0=ALU.mult, op1=ALU.mult),
                  r=[tab, tbb, sgb], w=[sb_])
            P.dma(ATT[hh, :, t0:t0 + n], st[:, :n], r=[sb_], w=[P.B(('ATT', hh, t0))], sem=ss)
        for hh in range(4):
            P.dma(kt[:], KT[hh], r=[P.B(('k', hh))], w=[ktb], sem=kts)
            P.dma(vt[:], VT[:, :, hh * 128:(hh + 1) * 128].rearrange("t p e -> p t e"), r=[P.B(('V', tt)) for tt in range(34)], w=[vtb], sem=vts)
            if own:
                P.dmad(qt[:, 256:256 + 2048], lambda off, hh=hh: QT[hh][:, bass.ds(256 + off, 2048)], r=[P.B(('q', hh))], w=[qtb], sem=qts)
                qblocks = [(256 + 512 * j, 512) for j in range(4)]
            else:
                P.dma(qt[:], QT[hh, :, 0:T], r=[P.B(('q', hh))], w=[qtb], sem=qts)
                qblocks = [b_ for b_ in BLKS if not (b_[0] == 0 and not ctx_out)]
            for (t0, n) in qblocks:
                attn_block(hh, t0, n)
        P.release()

    def phase_dn(l):
        ctx_out = l == 0
        P.mark()
        mk = P.sb([64, 6, 64], F32, "mk"); mkb = Buf(); mks = P.new_dsem()
        P.dma(mk[:], masks, w=[mkb], sem=mks)
        U = mk[:, 0, :]; L = mk[:, 1, :]
        ones64 = ones_f[0:64, 0:64]
        id64 = idf[0:64, 0:64]
        id64b = idb[0:64, 0:64]
        cfg = [dict(cum=U, m_strict=3, t_incl=4), dict(cum=L, m_strict=5, t_incl=2)]
        KP = 3

        def mk_slot(i):
            return dict(
                fqr=Ring(P, [64, 8, 64], BF16, 2, f"fq{i}"), tkr=Ring(P, [64, 512], BF16, 2, f"tok{i}"), gbr=Ring(P, [64, 16], F32, 2, f"gb{i}"),
                r0r=Ring(P, [64, 8, 64], F32, 1, f"R0{i}", dma=False), r1r=Ring(P, [64, 8, 64], F32, 1, f"R1{i}", dma=False),
                dcr=Ring(P, [64, 8, 64], F32, 1, f"dec{i}", dma=False), dtr=Ring(P, [64, 8, 64], F32, 1, f"decT{i}", dma=False),
                sc=Ring(P, [64, 32], F32, 2, f"sc{i}", dma=False), nr_=Ring(P, [64, 8, 64], BF16, 1, f"N{i}", dma=False), ntr=Ring(P, [64, 8, 64], BF16, 1, f"NT{i}", dma=False),
                p2r=Ring(P, [64, 8, 64], BF16, 2, f"P2{i}", dma=False), p2tr=Ring(P, [64, 8, 64], BF16, 2, f"P2T{i}", dma=False), ttr=Ring(P, [64, 8, 64], BF16, 2, f"TT{i}", dma=False),
                outb=Ring(P, [64, 2, 1024], BF16, 2, f"prepb{i}"), outf=Ring(P, [64, 2, 12], F32, 2, f"prepf{i}"),
                tmpf=Ring(P, [64, 8, 64], F32, 2, f"tmpf{i}", dma=False), kqr=Ring(P, [64, 4, 128], F32, 1, f"kq{i}", dma=False))
        slots = [mk_slot(i) for i in range(KP)]

        def _prep(c):
            SL = slots[c % KP]
            fq, fqb, fqs = SL['fqr'].next()
            tk, tkb, tks = SL['tkr'].next()
            gb, gbb, gbs = SL['gbr'].next()
            P.dma(fq[:], DNF.rearrange("i (h p) t -> p (i h) t", h=2)[:, :, c * 64:(c + 1) * 64], r=[P.B(('DNF', i)) for i in range(4)], w=[fqb], sem=fqs)
            P.dma(tk[:], DNT[c * 64:(c + 1) * 64, :], r=[P.B(('DNT', c // 2, i)) for i in range(2, 6)], w=[tkb], sem=tks)
            P.dma(gb[:], ZAB[c * 64:(c + 1) * 64, 256:272], r=[P.B(('Z', c // 2))], w=[gbb], sem=gbs)
            psA, psAb = P.ps()
            psAv = psA[0:64, :].rearrange("p (h x) -> p h x", x=128)
            for hh in range(4):
                P.pe(lambda e, hh=hh, fq=fq, psAv=psAv: e.matmul(psAv[:, hh, :], lhsT=fq[:, 4 + hh, :], rhs=fq[:, hh::4, :], start=True, stop=True),
                     r=[fqb], w=[psAb])
            yield
            kq, kqb, _ = SL['kqr'].next()
            P.act(lambda e, kq=kq, psA=psA: e.copy(out=kq[:].rearrange("p h x -> p (h x)"), in_=psA[0:64, :]), r=[psAb], w=[kqb])
            R0, R0b, _ = SL['r0r'].next()
            R1, R1b, _ = SL['r1r'].next()
            g8 = gb[:, 0:8]
            P.dve(lambda e, R0=R0, g8=g8: e.tensor_copy(out=R0[:], in_=bc(g8, [64, 8, 64])), r=[gbb], w=[R0b])
            for d in range(2):
                cm = cfg[d]['cum']
                P.pool(lambda e, R1=R1, d=d, cm=cm, g8=g8: e.tensor_tensor(out=R1[:, d * 4:(d + 1) * 4, :], in0=bc(g8[:, d * 4:(d + 1) * 4], [64, 4, 64]),
                                                                          in1=cm.unsqueeze(1).to_broadcast([64, 4, 64]), op=ALU.mult),
                       r=[gbb, mkb], w=[R1b])
            psD, psDb = P.ps()
            psT, psTb = P.ps()
            psDv = psD[0:64, :].rearrange("p (a x) -> p a x", x=64)
            psTv = psT[0:64, :].rearrange("p (a x) -> p a x", x=64)
            nR0, nR0b, _ = SL['tmpf'].next()
            nR1, nR1b, _ = SL['tmpf'].next()
            P.dve(lambda e, nR0=nR0, R0=R0: e.tensor_scalar(out=nR0[:], in0=R0[:], scalar1=-1.0, scalar2=None, op0=ALU.mult), r=[R0b], w=[nR0b])
            P.pool(lambda e, nR1=nR1, R1=R1: e.tensor_scalar(out=nR1[:], in0=R1[:], scalar1=-1.0, scalar2=None, op0=ALU.mult), r=[R1b], w=[nR1b])
            yield
            for d in range(2):
                cm = cfg[d]['cum']
                sl = slice(d * 4, (d + 1) * 4)
                P.pe(lambda e, cm=cm, sl=sl, R0=R0: e.matmul(psDv[:, sl, :], lhsT=cm, rhs=R0[:, sl, :], start=True, stop=False), r=[mkb, R0b], w=[psDb])
                P.pe(lambda e, sl=sl, nR1=nR1: e.matmul(psDv[:, sl, :], lhsT=ones64, rhs=nR1[:, sl, :], start=False, stop=False), r=[onesb, nR1b], w=[psDb])
                P.pe(lambda e, sl=sl, d=d: e.matmul(psDv[:, sl, :], lhsT=id64, rhs=mk[:, cfg[d]['m_strict'], :].unsqueeze(1).to_broadcast([64, 4, 64]), start=False, stop=True),
                     r=[idfb, mkb], w=[psDb])
                P.pe(lambda e, cm=cm, sl=sl, nR0=nR0: e.matmul(psTv[:, sl, :], lhsT=cm, rhs=nR0[:, sl, :], start=True, stop=False), r=[mkb, nR0b], w=[psTb])
                P.pe(lambda e, sl=sl, R1=R1: e.matmul(psTv[:, sl, :], lhsT=ones64, rhs=R1[:, sl, :], start=False, stop=False), r=[onesb, R1b], w=[psTb])
                P.pe(lambda e, sl=sl, d=d: e.matmul(psTv[:, sl, :], lhsT=id64, rhs=mk[:, cfg[d]['t_incl'], :].unsqueeze(1).to_broadcast([64, 4, 64]), start=False, stop=True),
                     r=[idfb, mkb], w=[psTb])
            yield
            dec, decb, _ = SL['dcr'].next()
            decT, decTb, _ = SL['dtr'].next()
            P.act(lambda e, dec=dec: e.activation(out=dec[:].rearrange("p a x -> p (a x)"), in_=psD[0:64, :], func=AF.Exp), r=[psDb], w=[decb])
            P.act(lambda e, decT=decT: e.activation(out=decT[:].rearrange("p a x -> p (a x)"), in_=psT[0:64, :], func=AF.Exp), r=[psTb], w=[decTb])
            psG, psGb = P.ps()
            for d in range(2):
                P.pe(lambda e, d=d: e.matmul(psG[0:64, d * 4:(d + 1) * 4], lhsT=cfg[d]['cum'], rhs=gb[:, d * 4:(d + 1) * 4], start=True, stop=True), r=[mkb, gbb], w=[psGb])
            P.pe(lambda e: e.matmul(psG[0:64, 8:16], lhsT=ones64, rhs=gb[:, 0:8], start=True, stop=True), r=[onesb, gbb], w=[psGb])
            s_, sb2, _ = SL['sc'].next()
            of, ofb, ofs = SL['outf'].next()
            ofv = of[:].rearrange("p d (k h) -> p d k h", h=4)
            P.act(lambda e, s_=s_, psG=psG: e.copy(out=s_[:, 0:16], in_=psG[0:64, 0:16]), r=[psGb], w=[sb2])
            P.dve(lambda e, s_=s_: e.tensor_tensor(out=s_[:, 16:24], in0=s_[:, 8:16], in1=s_[:, 0:8], op=ALU.subtract), r=[sb2], w=[sb2])
            P.act(lambda e, s_=s_: e.activation(out=s_[:, 0:24], in_=s_[:, 0:24], func=AF.Exp), r=[sb2], w=[sb2])
            s3 = s_[:, 0:24].rearrange("p (k d h) -> p k d h", d=2, h=4)
            beta = gb[:, 8:16].rearrange("p (d h) -> p d h", h=4)
            P.dve(lambda e, ofv=ofv, s3=s3, beta=beta: e.tensor_tensor(out=ofv[:, :, 0, :], in0=s3[:, 0, :, :], in1=beta, op=ALU.mult), r=[sb2, gbb], w=[ofb])
            P.pool(lambda e, ofv=ofv, s3=s3: e.tensor_copy(out=ofv[:, :, 1, :], in_=s3[:, 0, :, :]), r=[sb2], w=[ofb])
            P.pool(lambda e, ofv=ofv, s3=s3: e.tensor_copy(out=ofv[:, :, 2, :], in_=s3[:, 1, :, :]), r=[sb2], w=[ofb])
            yield
            ob, obb, obs = SL['outb'].next()
            obv = ob[:].rearrange("p d (k h x) -> p d k h x", k=4, h=4)
            ktok = tk[:, 0:256].rearrange("p (h x) -> p h x", x=64)
            vtok = tk[:, 256:512].rearrange("p (h x) -> p h x", x=64)
            for d in range(2):
                P.dve(lambda e, d=d, obv=obv, s_=s_, ktok=ktok: e.tensor_tensor(out=obv[:, d, 2, :, :], in0=ktok, in1=bc(s_[:, 16 + d * 4:20 + d * 4], [64, 4, 64]), op=ALU.mult),
                      r=[tkb, sb2], w=[obb])
                P.pool(lambda e, d=d, obv=obv, vtok=vtok: e.tensor_tensor(out=obv[:, d, 3, :, :], in0=vtok, in1=bc(gb[:, 8 + d * 4:12 + d * 4], [64, 4, 64]), op=ALU.mult),
                       r=[tkb, gbb], w=[obb])
                P.dve(lambda e, d=d, obv=obv, decT=decT: e.tensor_tensor(out=obv[:, d, 1, :, :], in0=kq[:, :, 0:64], in1=decT[:, d * 4:(d + 1) * 4, :], op=ALU.mult),
                      r=[kqb, decTb], w=[obb])
            N, Nb, _ = SL['nr_'].next()
            tm, tmb, _ = SL['tmpf'].next()
            for d in range(2):
                P.dve(lambda e, d=d, tm=tm, dec=dec: e.tensor_tensor(out=tm[:, d * 4:(d + 1) * 4, :], in0=kq[:, :, 64:128], in1=dec[:, d * 4:(d + 1) * 4, :], op=ALU.mult),
                      r=[kqb, decb], w=[tmb])
            P.dve(lambda e, N=N, tm=tm: e.scalar_tensor_tensor(out=N[:], in0=tm[:], scalar=-1.0, in1=bc(gb[:, 8:16], [64, 8, 64]), op0=ALU.mult, op1=ALU.mult),
                  r=[tmb, gbb], w=[Nb])
            yield
            psN, psNb = P.ps()
            psNv = psN[0:64, 0:256].bitcast(BF16).rearrange("p (a x) -> p a x", x=64)
            for a in range(8):
                P.pe(lambda e, a=a, N=N: e.transpose(out=psNv[:, a, :], in_=N[:, a, :], identity=id64b), r=[Nb, idbb], w=[psNb])
            yield
            NT, NTb, _ = SL['ntr'].next()
            P.act(lambda e, NT=NT: e.copy(out=NT[:], in_=psNv), r=[psNb], w=[NTb])
            TT, TTb, _ = SL['ttr'].next()
            P.pool(lambda e, TT=TT, NT=NT: e.tensor_tensor(out=TT[:], in0=NT[:], in1=id64b.unsqueeze(1).to_broadcast([64, 8, 64]), op=ALU.add), r=[NTb, idbb], w=[TTb])
            yield
            Pk, Pkb, PkT, PkTb = N, Nb, NT, NTb
            for lev in range(5):
                ps1, ps1b = P.ps()
                ps1v = ps1[0:64, :].rearrange("p (a x) -> p a x", x=64)
                for a in range(8):
                    P.pe(lambda e, a=a, PkT=PkT, Pk=Pk, ps1v=ps1v: e.matmul(ps1v[:, a, :], lhsT=PkT[:, a, :], rhs=Pk[:, a, :], start=True, stop=True), r=[PkTb, Pkb], w=[ps1b])
                if lev < 4:
                    ps2, ps2b = P.ps()
                    ps2v = ps2[0:64, :].rearrange("p (a x) -> p a x", x=64)
                    for a in range(8):
                        P.pe(lambda e, a=a, PkT=PkT, Pk=Pk, ps2v=ps2v: e.matmul(ps2v[:, a, :], lhsT=Pk[:, a, :], rhs=PkT[:, a, :], start=True, stop=True), r=[PkTb, Pkb], w=[ps2b])
                yield
                Pn, Pnb, _ = SL['p2r'].next()
                P.act(lambda e, Pn=Pn, ps1=ps1: e.copy(out=Pn[:].rearrange("p a x -> p (a x)"), in_=ps1[0:64, :]), r=[ps1b], w=[Pnb])
                if lev < 4:
                    PnT, PnTb, _ = SL['p2tr'].next()
                    P.dve(lambda e, PnT=PnT, ps2=ps2: e.tensor_copy(out=PnT[:].rearrange("p a x -> p (a x)"), in_=ps2[0:64, :]), r=[ps2b], w=[PnTb])
                yield
                ps3, ps3b = P.ps()
                ps3v = ps3[0:64, :].rearrange("p (a x) -> p a x", x=64)
                for a in range(8):
                    P.pe(lambda e, a=a, Pn=Pn, TT=TT, ps3v=ps3v: e.matmul(ps3v[:, a, :], lhsT=Pn[:, a, :], rhs=TT[:, a, :], start=True, stop=True), r=[Pnb, TTb], w=[ps3b])
                yield
                TT2, TT2b, _ = SL['ttr'].next()
                P.dve(lambda e, TT2=TT2, TT=TT, ps3=ps3: e.tensor_tensor(out=TT2[:].rearrange("p a x -> p (a x)"), in0=ps3[0:64, :], in1=TT[:].rearrange("p a x -> p (a x)"), op=ALU.add),
                      r=[ps3b, TTb], w=[TT2b])
                TT, TTb = TT2, TT2b
                Pk, Pkb = Pn, Pnb
                if lev < 4:
                    PkT, PkTb = PnT, PnTb
                yield
            for d in range(2):
                P.pool(lambda e, d=d, obv=obv, TT=TT: e.tensor_copy(out=obv[:, d, 0, :, :], in_=TT[:, d * 4:(d + 1) * 4, :]), r=[TTb], w=[obb])
            P.dma(PREPB[:, c].rearrange("d p x -> p d x"), ob[:], r=[obb], w=[P.B(('PB', c))], sem=obs)
            P.dma(PREPF[:, c].rearrange("d p x -> p d x"), of[:], r=[ofb], w=[P.B(('PF', c))], sem=ofs)
        def run_interleaved(gens_iter, width):
            active = []
            it = iter(gens_iter)
            done = False
            while True:
                while len(active) < width and not done:
                    try:
                        active.append(next(it))
                    except StopIteration:
                        done = True
                if not active:
                    break
                for g in list(active):
                    try:
                        next(g)
                    except StopIteration:
                        active.remove(g)
        run_interleaved((_prep(c) for c in range(NCH)), KP)
        P.release()

        P.mark()
        order = [list(range(NCH)), [3, 2, 1, 0] + list(range(NCH - 1, 3, -1))]
        DNFv = DNF.rearrange("i (h p) t -> p (i h) t", h=2)

        def mk_dir(d):
            return dict(S=P.sb([64, 4, 64], F32, f"S{d}"), Sb=Buf(), Sh=P.sb([64, 4, 64], BF16, f"Sh{d}"), Shb=Buf(),
                        pbr=Ring(P, [64, 1024], BF16, 3, f"pb{d}"), pfr=Ring(P, [64, 12], F32, 3, f"pf{d}"), fqr=Ring(P, [64, 8, 64], BF16, 3, f"fq2{d}"),
                        xr=Ring(P, [64, 4, 64], BF16, 2, f"X{d}", dma=False), vnr=Ring(P, [64, 4, 64], BF16, 2, f"vn{d}", dma=False),
                        t1r=Ring(P, [64, 4, 64], F32, 2, f"st1{d}", dma=False), osr=Ring(P, [64, 4, 64], F32, 3, f"os{d}"))
        dirs = [mk_dir(d) for d in range(2)]

        def v4(ps):
            return ps[0:64, 0:256].rearrange("p (a x) -> p a x", x=64)

        def _scan_dir(d):
            D = dirs[d]
            S, Sb_, Sh, Shb = D['S'], D['Sb'], D['Sh'], D['Shb']
            P.pool(lambda e: e.memset(S[:], 0.0), w=[Sb_])
            P.pool(lambda e: e.memset(Sh[:], 0.0), w=[Shb])
            for s_i in range(NCH):
                c = order[d][s_i]
                pb_, pbb_, pbs = D['pbr'].next()
                pf, pfb, pfs = D['pfr'].next()
                fq, fqb, fqs = D['fqr'].next()
                P.dma(pb_[:], PREPB[d, c], r=[P.B(('PB', c))], w=[pbb_], sem=pbs)
                P.dma(pf[:], PREPF[d, c], r=[P.B(('PF', c))], w=[pfb], sem=pfs)
                P.dma(fq[:], DNFv[:, :, c * 64:(c + 1) * 64], r=[P.B(('DNF', i)) for i in range(4)], w=[fqb], sem=fqs)
                pbv = pb_[:].rearrange("p (k h x) -> p k h x", k=4, h=4)
                pfv = pf[:].rearrange("p (k h) -> p k h", h=4)
                psK, psKb = P.ps()
                psQ, psQb = P.ps()
                psKv, psQv = v4(psK), v4(psQ)
                for hh in range(4):
                    P.pe(lambda e, hh=hh, fq=fq, psKv=psKv: e.matmul(psKv[:, hh, :], lhsT=fq[:, 4 + hh, :], rhs=Sh[:, hh, :], start=True, stop=True), r=[fqb, Shb], w=[psKb])
                for hh in range(4):
                    P.pe(lambda e, hh=hh, fq=fq, psQv=psQv: e.matmul(psQv[:, hh, :], lhsT=fq[:, hh, :], rhs=Sh[:, hh, :], start=True, stop=True), r=[fqb, Shb], w=[psQb])
                yield
                t1, t1b, _ = D['t1r'].next()
                X, Xb, _ = D['xr'].next()
                os_, osb, oss = D['osr'].next()
                P.dve(lambda e, t1=t1, psKv=psKv, pfv=pfv: e.tensor_tensor(out=t1[:], in0=psKv, in1=bc(pfv[:, 0, :], [64, 4, 64]), op=ALU.mult), r=[psKb, pfb], w=[t1b])
                P.dve(lambda e, t1=t1, X=X, pbv=pbv: e.tensor_tensor(out=X[:], in0=pbv[:, 3, :, :], in1=t1[:], op=ALU.subtract), r=[t1b, pbb_], w=[Xb])
                P.dve(lambda e, os_=os_, psQv=psQv, pfv=pfv: e.tensor_tensor(out=os_[:], in0=psQv, in1=bc(pfv[:, 1, :], [64, 4, 64]), op=ALU.mult), r=[psQb, pfb], w=[osb])
                yield
                psV, psVb = P.ps()
                psVv = v4(psV)
                for hh in range(4):
                    P.pe(lambda e, hh=hh, pbv=pbv, X=X, psVv=psVv: e.matmul(psVv[:, hh, :], lhsT=pbv[:, 0, hh, :], rhs=X[:, hh, :], start=True, stop=True), r=[pbb_, Xb], w=[psVb])
                yield
                vn, vnb, _ = D['vnr'].next()
                P.act(lambda e, vn=vn, psVv=psVv: e.copy(out=vn[:], in_=psVv), r=[psVb], w=[vnb])
                yield
                psS, psSb = P.ps()
                psO, psOb = P.ps()
                psSv, psOv = v4(psS), v4(psO)
                for hh in range(4):
                    P.pe(lambda e, hh=hh, pbv=pbv, vn=vn, psSv=psSv: e.matmul(psSv[:, hh, :], lhsT=pbv[:, 2, hh, :], rhs=vn[:, hh, :], start=True, stop=True), r=[pbb_, vnb], w=[psSb])
                for hh in range(4):
                    P.pe(lambda e, hh=hh, pbv=pbv, vn=vn, psOv=psOv: e.matmul(psOv[:, hh, :], lhsT=pbv[:, 1, hh, :], rhs=vn[:, hh, :], start=True, stop=True), r=[pbb_, vnb], w=[psOb])
                yield
                P.dve(lambda e, pfv=pfv: e.tensor_tensor(out=S[:], in0=S[:], in1=bc(pfv[:, 2, :], [64, 4, 64]), op=ALU.mult), r=[Sb_, pfb], w=[Sb_])
                P.dve(lambda e, psSv=psSv: e.tensor_tensor(out=S[:], in0=S[:], in1=psSv, op=ALU.add), r=[Sb_, psSb], w=[Sb_])
                P.act(lambda e: e.copy(out=Sh[:], in_=S[:]), r=[Sb_], w=[Shb])
                P.dve(lambda e, os_=os_, psOv=psOv: e.tensor_tensor(out=os_[:], in0=os_[:], in1=psOv, op=ALU.add), r=[osb, psOb], w=[osb])
                if not (c < 4 and not ctx_out):
                    P.dma(OSC[d, c * 64:(c + 1) * 64, :], os_[:].rearrange("p a x -> p (a x)"), r=[osb], w=[P.B(('O', d, c))], sem=oss)
                yield
        run_interleaved((_scan_dir(d) for d in range(2)), 2)
        P.release()

        P.mark()
        o0r = Ring(P, [128, 2, 256], F32, 2, "o0")
        zr = Ring(P, [128, 256], F32, 2, "zr")
        sqr = Ring(P, [128, 256], F32, 2, "sqo")
        ssr = Ring(P, [128, 4], F32, 2, "sso")
        yr = Ring(P, [128, 256], BF16, 2, "yo")
        ngt = P.sb([128, 256], F32, "ngt"); ngtb = Buf(); ngts = P.new_dsem()
        e64 = P.sb([128, 1], F32, "e64"); e64b = Buf()
        P.pool(lambda e: e.memset(e64[:], EPS), w=[e64b])
        P.dma(ngt[:], dnng[l].partition_broadcast(128), w=[ngtb], sem=ngts)
        tst = Ring(P, [128, 2, 128], BF16, 2, "tst2")
        def _og(tt):
            if tt < 2 and not ctx_out:
                return
            o0, o0b, o0s = o0r.next()
            z, zb, zs = zr.next()
            P.dma(o0[:], OSC[:, tt * 128:(tt + 1) * 128, :].rearrange("d p x -> p d x"), r=[P.B(('O', d, 2 * tt + k)) for d in range(2) for k in range(2)], w=[o0b], sem=o0s)
            P.dma(z[:], ZAB[tt * 128:(tt + 1) * 128, 0:256], r=[P.B(('Z', tt))], w=[zb], sem=zs)
            P.pool(lambda e, o0=o0: e.tensor_tensor(out=o0[:, 0, :], in0=o0[:, 0, :], in1=o0[:, 1, :], op=ALU.add), r=[o0b], w=[o0b])
            sq, sqb, _ = sqr.next()
            P.act(lambda e, sq=sq, o0=o0: e.activation(out=sq[:], in_=o0[:, 0, :], func=AF.Square), r=[o0b], w=[sqb])
            ss_, ssb, _ = ssr.next()
            P.dve(lambda e, ss_=ss_, sq=sq: e.tensor_reduce(out=ss_[:], in_=sq[:].rearrange("p (h x) -> p h x", x=64), axis=AX.X, op=ALU.add), r=[sqb], w=[ssb])
            P.act(lambda e, ss_=ss_: e.activation(out=ss_[:], in_=ss_[:], func=AF.Sqrt, bias=e64[:], scale=1.0 / 64), r=[ssb, e64b], w=[ssb])
            P.dve(lambda e, ss_=ss_: e.reciprocal(out=ss_[:], in_=ss_[:]), r=[ssb], w=[ssb])
            P.dve(lambda e, sq=sq, o0=o0, ss_=ss_: e.tensor_tensor(out=sq[:].rearrange("p (h x) -> p h x", x=64), in0=o0[:, 0, :].rearrange("p (h x) -> p h x", x=64), in1=bc(ss_[:, 0:4], [128, 4, 64]), op=ALU.mult),
                  r=[o0b, ssb], w=[sqb])
            P.pool(lambda e, sq=sq: e.tensor_tensor(out=sq[:], in0=sq[:], in1=ngt[:], op=ALU.mult), r=[sqb, ngtb], w=[sqb])
            y, yb, _ = yr.next()
            P.dve(lambda e, y=y, sq=sq, z=z: e.tensor_tensor(out=y[:], in0=sq[:], in1=z[:], op=ALU.mult), r=[sqb, zb], w=[yb])
            st, sb_, ss2 = tst.next()
            for k in range(2):
                ps, pb = P.ps()
                psv = ps[:, 0:64].bitcast(BF16)
                P.pe(lambda e, psv=psv, y=y, k=k: e.transpose(out=psv, in_=y[:, k * 128:(k + 1) * 128], identity=idb[:]), r=[yb, idbb], w=[pb])
                P.act(lambda e, psv=psv, st=st, k=k: e.copy(out=st[:, k, :], in_=psv), r=[pb], w=[sb_])
            P.dma(DNY[:, :, tt * 128:(tt + 1) * 128].rearrange("k p t -> p k t"), st[:], r=[sb_], w=[P.B(('DNY', tt))], sem=ss2)
        for tt in range(34):
            _og(tt)
        P.release()

    def phase_merge(l):
        ctx_out = l == 0
        P.mark()
        wb = P.sb([128, 8, 1024], BF16, "wbr"); wbb = Buf(); wbs = P.new_dsem()
        wo = P.sb([128, 8, 1024], BF16, "wo"); wob = Buf(); wos = P.new_dsem()
        P.dma(wb[:], w_br[l].rearrange("c p n -> p c n"), w=[wbb], sem=wbs, q='pool')
        P.dma(wo[:], w_out[l].rearrange("c p n -> p c n"), w=[wob], sem=wos, q='pool')
        inr = Ring(P, [128, 8, 512], BF16, 2, "min")
        gr = Ring(P, [128, 24, 512], BF16, 2, "gin")
        hr = Ring(P, [128, 8, 512], F32, 2, "hin")
        mixr = Ring(P, [128, 8, 512], BF16, 2, "mix")
        tr_ = Ring(P, [128, 512], F32, 6, "mt")
        src = hsrc_of(l)
        own = SPLIT and l == 1

        def _mb(t0, n):
            if t0 == 0 and not ctx_out:
                return
            s_ = 1 if t0 == 0 else 0
            xin, xb, xs = inr.next()
            g, gb_, gs = gr.next()
            h, hb_, hs = hr.next()
            P.dma(xin[:, 0:4, :n], ATT[:, :, t0:t0 + n].rearrange("c p t -> p c t"), r=[P.B(('ATT', hh, t0)) for hh in range(4)], w=[xb], sem=xs)
            if own:
                cands = (t0, t0 + 2048)
                P.dmad(xin[:, 4:6, :n], lambda off: DNY[:, :, bass.ds(t0 + off, n)].rearrange("c p t -> p c t"),
                       r=[P.B(('DNY', tt)) for tc in cands for tt in range(tc // 128, (tc + n) // 128)], w=[xb], sem=xs)
                P.dmad(xin[:, 6:8, :n], lambda off: POOLT[:, :, bass.ds(t0 + off, n)].rearrange("c p t -> p c t"), r=[P.B(('POOL', i)) for i in range(2)], w=[xb], sem=xs)
                P.dmad(g[:, :, :n], lambda off: GT[:, :, bass.ds(t0 + off, n)].rearrange("c p t -> p c t"), r=[P.B(('G', j, tc)) for j in range(24) for tc in cands], w=[gb_], sem=gs)
                P.dmad(h[:, :, :n], lambda off: src[:, :, bass.ds(t0 + off, n)].rearrange("c p t -> p c t"), r=[P.B(('H', tc)) for tc in cands], w=[hb_], sem=hs)
            else:
                P.dma(xin[:, 4:6, :n], DNY[:, :, t0:t0 + n].rearrange("c p t -> p c t"), r=[P.B(('DNY', tt)) for tt in range(t0 // 128, (t0 + n) // 128)], w=[xb], sem=xs)
                P.dma(xin[:, 6:8, :n], POOLT[:, :, t0:t0 + n].rearrange("c p t -> p c t"), r=[P.B(('POOL', i)) for i in range(2)], w=[xb], sem=xs)
                P.dma(g[:, :, :n], GT[:, :, t0:t0 + n].rearrange("c p t -> p c t"), r=[P.B(('G', j, t0)) for j in range(24)], w=[gb_], sem=gs)
                P.dma(h[:, :, :n], src[:, :, t0:t0 + n].rearrange("c p t -> p c t"), r=[P.B(('H', t0))], w=[hb_], sem=hs)
            mix, mixb, _ = mixr.next()
            for c in range(8):
                cs = slice(c * 128, (c + 1) * 128)
                psa, psab = P.ps()
                for k in range(4):
                    P.pe(lambda e, k=k, cs=cs, psa=psa, xin=xin: e.matmul(psa[:, :n], lhsT=wb[:, k, cs], rhs=xin[:, k, :n], start=(k == 0), stop=(k == 3)), r=[wbb, xb], w=[psab])
                psd, psdb = P.ps()
                for k in range(2):
                    P.pe(lambda e, k=k, cs=cs, psd=psd, xin=xin: e.matmul(psd[:, :n], lhsT=wb[:, 4 + k, cs], rhs=xin[:, 4 + k, :n], start=(k == 0), stop=(k == 1)), r=[wbb, xb], w=[psdb])
                psp, pspb = P.ps()
                for k in range(2):
                    P.pe(lambda e, k=k, cs=cs, psp=psp, xin=xin: e.matmul(psp[:, :n], lhsT=wb[:, 6 + k, cs], rhs=xin[:, 6 + k, :n], start=(k == 0), stop=(k == 1)), r=[wbb, xb], w=[pspb])
                t1, t1b, _ = tr_.next()
                t2, t2b, _ = tr_.next()
                t3, t3b, _ = tr_.next()
                P.dve(lambda e, t1=t1, psa=psa, g=g, c=c: e.tensor_tensor(out=t1[:, :n], in0=psa[:, :n], in1=g[:, c, :n], op=ALU.mult), r=[psab, gb_], w=[t1b])
                P.dve(lambda e, t2=t2, psd=psd, g=g, c=c: e.tensor_tensor(out=t2[:, :n], in0=psd[:, :n], in1=g[:, 8 + c, :n], op=ALU.mult), r=[psdb, gb_], w=[t2b])
                P.dve(lambda e, t3=t3, psp=psp, g=g, c=c: e.tensor_tensor(out=t3[:, :n], in0=psp[:, :n], in1=g[:, 16 + c, :n], op=ALU.mult), r=[pspb, gb_], w=[t3b])
                P.pool(lambda e, t1=t1, t2=t2: e.tensor_tensor(out=t1[:, :n], in0=t1[:, :n], in1=t2[:, :n], op=ALU.add), r=[t1b, t2b], w=[t1b])
                P.pool(lambda e, t1=t1, t3=t3, mix=mix, c=c: e.tensor_tensor(out=mix[:, c, :n], in0=t1[:, :n], in1=t3[:, :n], op=ALU.add), r=[t1b, t3b], w=[mixb])
            for c in range(8):
                cs = slice(c * 128, (c + 1) * 128)
                psy, psyb = P.ps()
                for k in range(8):
                    P.pe(lambda e, k=k, cs=cs, psy=psy, mix=mix: e.matmul(psy[:, :n], lhsT=wo[:, k, cs], rhs=mix[:, k, :n], start=(k == 0), stop=(k == 7)), r=[wob, mixb], w=[psyb])
                P.dve(lambda e, c=c, psy=psy, h=h, s_=s_: e.scalar_tensor_tensor(out=h[:, c, :n], in0=psy[:, :n], scalar=mod[:, s_, 16 + c:17 + c], in1=h[:, c, :n], op0=ALU.mult, op1=ALU.add),
                      r=[psyb, modb, hb_], w=[hb_])
            P.dma(H[:, :, t0:t0 + n].rearrange("c p t -> p c t"), h[:, :, :n], r=[hb_], w=[P.B(('H', t0))], sem=hs)
        for (t0, n) in ([(256 + 512 * j, 512) for j in range(4)] if own else BLKS):
            _mb(t0, n)
        P.release()

    def phase_ffn(l, final):
        moe = (l == 1)
        ctx_out = l == 0
        NF = 28 if moe else 22
        P.mark()
        SBK = 1024
        hb_t = P.sb([128, 8, SBK], F32, "hsb"); hbb = Buf(); hbs = P.new_dsem()
        u2 = P.sb([128, 8, SBK], BF16, "u2"); u2b = Buf()
        hid = P.sb([128, NF, SBK], BF16, "hid"); hidb = Buf()
        rs = ln_rings()
        w13 = Ring(P, [128, 2, 8, 512], BF16, 2, "w13")
        w2r = Ring(P, [128, 4, 1024], BF16, 2, "w2")
        sr = Ring(P, [128, 512], F32, 4, "sil", dma=False)
        if moe:
            rwt = P.sb([128, 8, 8], F32, "rwt"); rwb = Buf(); rws = P.new_dsem()
            P.dma(rwt[:], rw, w=[rwb], sem=rws)
            selt = P.sb([8, 8, 128], F32, "selt"); selb = Buf()
            P.dma(selt[:], selc, w=[selb], sem=rws)
            lg = P.sb([128, 8, 8], F32, "lg"); lgb = Buf()
            cmb = P.sb([128, 8, 8], F32, "cmb"); cmbb = Buf()
            combT = P.sb([8, SBK], F32, "combT"); combTb = Buf()
            cb = P.sb([128, SBK], F32, "cb"); cbb = Buf()
            sm = Ring(P, [128, 8], F32, 4, "sm")
            sm1 = Ring(P, [128, 1], F32, 6, "sm1")
        sblocks = ([(0, 256)] if (ctx_out and not final) else []) + [(256 + SBK * i, SBK) for i in range((TOUT if (SPLIT and l == 1) else TL) // SBK)]
        if final:
            pass
        for (T0, NS) in sblocks:
            s_ = 1 if T0 == 0 else 0
            P.dma(hb_t[:, :, :NS], H[:, :, T0:T0 + NS].rearrange("c p t -> p c t"), r=[P.B(('H', t)) for t in range(T0, T0 + NS, 512)], w=[hbb], sem=hbs)
            nb = (NS + 511) // 512
            for bi in range(nb):
                n = min(512, NS - bi * 512)
                o = bi * 512
                if moe:
                    psRs = [P.ps() for _ in range(4)]

                    def hook(c, tmp, tb, tsem, psRs=psRs, n=n):
                        for tt in range(n // 128):
                            psR, psRb = psRs[tt]
                            P.pe(lambda e, tt=tt, c=c, tmp=tmp, psR=psR: e.matmul(psR[:, 0:8], lhsT=tmp[:, tt * 128:(tt + 1) * 128], rhs=rwt[:, c, :],
                                                                                  start=(c == 0), stop=(c == 7)), r=[tb, rwb], w=[psRb])
                else:
                    hook = None
                ln_block(hb_t[:, :, o:o + n], hbb, n, lambda c, s_=s_: gain[:, 1, s_, c:c + 1], lambda c, s_=s_: mod[:, s_, 24 + c:25 + c],
                         u2, u2b, o, rs, f32hook=hook)
                if moe:
                    for tt in range(4):
                        psR, psRb = psRs[tt]
                        P.act(lambda e, psR=psR, bi=bi, tt=tt: e.copy(out=lg[:, bi * 4 + tt, :], in_=psR[:, 0:8]), r=[psRb], w=[lgb])
            if moe:
                for tt in range(NS // 128):
                    m1, m1b, _ = sm1.next()
                    m2, m2b, _ = sm1.next()
                    dn_, dnb, _ = sm1.next()
                    l2_, l2b_, _ = sm.next()
                    w_, wb2, _ = sm.next()
                    lgt = lg[:, tt, :]
                    P.dve(lambda e, m1=m1, lgt=lgt: e.tensor_reduce(out=m1[:], in_=lgt, axis=AX.X, op=ALU.max), r=[lgb], w=[m1b])
                    P.dve(lambda e, l2_=l2_, lgt=lgt, m1=m1: e.tensor_scalar(out=l2_[:], in0=lgt, scalar1=m1[:, 0:1], scalar2=-1e30, op0=ALU.is_equal, op1=ALU.mult), r=[lgb, m1b], w=[l2b_])
                    P.dve(lambda e, l2_=l2_, lgt=lgt: e.tensor_tensor(out=l2_[:], in0=l2_[:], in1=lgt, op=ALU.add), r=[l2b_, lgb], w=[l2b_])
                    P.dve(lambda e, m2=m2, l2_=l2_: e.tensor_reduce(out=m2[:], in_=l2_[:], axis=AX.X, op=ALU.max), r=[l2b_], w=[m2b])
                    P.dve(lambda e, l2_=l2_, lgt=lgt, m2=m2: e.tensor_scalar(out=l2_[:], in0=lgt, scalar1=m2[:, 0:1], scalar2=None, op0=ALU.is_ge), r=[lgb, m2b, l2b_], w=[l2b_])
                    P.dve(lambda e, w_=w_, lgt=lgt, m1=m1: e.tensor_scalar(out=w_[:], in0=lgt, scalar1=m1[:, 0:1], scalar2=None, op0=ALU.subtract), r=[lgb, m1b], w=[wb2])
                    P.act(lambda e, w_=w_: e.activation(out=w_[:], in_=w_[:], func=AF.Exp), r=[wb2], w=[wb2])
                    P.dve(lambda e, w_=w_, l2_=l2_: e.tensor_tensor(out=w_[:], in0=w_[:], in1=l2_[:], op=ALU.mult), r=[wb2, l2b_], w=[wb2])
                    P.dve(lambda e, dn_=dn_, w_=w_: e.tensor_reduce(out=dn_[:], in_=w_[:], axis=AX.X, op=ALU.add), r=[wb2], w=[dnb])
                    P.dve(lambda e, dn_=dn_: e.reciprocal(out=dn_[:], in_=dn_[:]), r=[dnb], w=[dnb])
                    P.dve(lambda e, tt=tt, w_=w_, dn_=dn_: e.tensor_scalar(out=cmb[:, tt, :], in0=w_[:], scalar1=dn_[:, 0:1], scalar2=None, op0=ALU.mult), r=[wb2, dnb], w=[cmbb])
                for half in range(NS // 512):
                    psC, psCb = P.ps()
                    for tq in range(4):
                        tt = half * 4 + tq
                        P.pe(lambda e, tt=tt, tq=tq, psC=psC: e.transpose(out=psC[0:8, tq * 128:(tq + 1) * 128], in_=cmb[:, tt, :], identity=idf[:]), r=[cmbb, idfb], w=[psCb])
                    P.act(lambda e, half=half, psC=psC: e.copy(out=combT[:, half * 512:(half + 1) * 512], in_=psC[0:8, :]), r=[psCb], w=[combTb])
            for ex in range(8 if moe else 1):
                if moe:
                    W1 = mw1[ex]; W3 = mw3[ex]; W2 = mw2[ex]
                    for half in range(NS // 512):
                        psB, psBb = P.ps()
                        P.pe(lambda e, ex=ex, half=half, psB=psB: e.matmul(psB[:, :], lhsT=selt[:, ex, :], rhs=combT[:, half * 512:(half + 1) * 512], start=True, stop=True), r=[selb, combTb], w=[psBb])
                        P.act(lambda e, half=half, psB=psB: e.copy(out=cb[:, half * 512:(half + 1) * 512], in_=psB[:, :]), r=[psBb], w=[cbb])
                else:
                    W1 = fw1; W3 = fw3; W2 = fw2
                HC = NF * 128
                for c0 in range(0, HC, 512):
                    gw = min(512, HC - c0)
                    wt, wtb, wts = w13.next()
                    P.dma(wt[:, 0, :, :gw], W1[:, :, c0:c0 + gw].rearrange("c p n -> p c n"), w=[wtb], sem=wts, q='pool')
                    P.dma(wt[:, 1, :, :gw], W3[:, :, c0:c0 + gw].rearrange("c p n -> p c n"), w=[wtb], sem=wts, q='pool')
                    for j in range(gw // 128):
                        f = c0 // 128 + j
                        for bi in range(nb):
                            n = min(512, NS - bi * 512)
                            o = bi * 512
                            ps1, ps1b = P.ps()
                            for c in range(8):
                                P.pe(lambda e, c=c, j=j, ps1=ps1, wt=wt, o=o, n=n: e.matmul(ps1[:, :n], lhsT=wt[:, 0, c, j * 128:(j + 1) * 128], rhs=u2[:, c, o:o + n], start=(c == 0), stop=(c == 7)), r=[wtb, u2b], w=[ps1b])
                            ps3, ps3b = P.ps()
                            for c in range(8):
                                P.pe(lambda e, c=c, j=j, ps3=ps3, wt=wt, o=o, n=n: e.matmul(ps3[:, :n], lhsT=wt[:, 1, c, j * 128:(j + 1) * 128], rhs=u2[:, c, o:o + n], start=(c == 0), stop=(c == 7)), r=[wtb, u2b], w=[ps3b])
                            sl, slb, _ = sr.next()
                            P.act(lambda e, sl=sl, ps1=ps1, n=n: e.activation(out=sl[:, :n], in_=ps1[:, :n], func=AF.Silu), r=[ps1b], w=[slb])
                            if moe:
                                g3, g3b, _ = sr.next()
                                P.dve(lambda e, g3=g3, ps3=ps3, o=o, n=n: e.tensor_tensor(out=g3[:, :n], in0=ps3[:, :n], in1=cb[:, o:o + n], op=ALU.mult), r=[ps3b, cbb], w=[g3b])
                                P.dve(lambda e, sl=sl, g3=g3, f=f, o=o, n=n: e.tensor_tensor(out=hid[:, f, o:o + n], in0=sl[:, :n], in1=g3[:, :n], op=ALU.mult), r=[slb, g3b], w=[hidb])
                            else:
                                P.dve(lambda e, sl=sl, ps3=ps3, f=f, o=o, n=n: e.tensor_tensor(out=hid[:, f, o:o + n], in0=sl[:, :n], in1=ps3[:, :n], op=ALU.mult), r=[slb, ps3b], w=[hidb])
                for k0 in range(0, NF, 4):
                    kg = min(4, NF - k0)
                    w2, w2b, w2s = w2r.next()
                    P.dma(w2[:, :kg, :], W2[k0:k0 + kg].rearrange("k p n -> p k n"), w=[w2b], sem=w2s, q='pool')
                    for c in range(8):
                        for bi in range(nb):
                            n = min(512, NS - bi * 512)
                            o = bi * 512
                            pso, psob = P.ps()
                            for k in range(kg):
                                P.pe(lambda e, k=k, c=c, pso=pso, w2=w2, o=o, n=n, k0=k0, kg=kg: e.matmul(pso[:, :n], lhsT=w2[:, k, c * 128:(c + 1) * 128], rhs=hid[:, k0 + k, o:o + n], start=(k == 0), stop=(k == kg - 1)), r=[w2b, hidb], w=[psob])
                            P.dve(lambda e, c=c, pso=pso, o=o, n=n, s_=s_: e.scalar_tensor_tensor(out=hb_t[:, c, o:o + n], in0=pso[:, :n], scalar=mod[:, s_, 40 + c:41 + c], in1=hb_t[:, c, o:o + n], op0=ALU.mult, op1=ALU.add),
                                  r=[psob, modb, hbb], w=[hbb])
            if not final:
                for bi in range(nb):
                    n = min(512, NS - bi * 512)
                    P.dma(H[:, :, T0 + bi * 512:T0 + bi * 512 + n].rearrange("c p t -> p c t"), hb_t[:, :, bi * 512:bi * 512 + n], r=[hbb], w=[P.B(('H', T0 + bi * 512))], sem=hbs)
            else:
                for bi in range(nb):
                    o = bi * 512
                    pos = T0 - 256 + o

                    def hookf(c, tmp, tb, tsem, pos=pos):
                        P.dma(yT[c, :, pos:pos + 512], tmp[:, :], r=[tb], w=[P.B(('Y', pos, c))], sem=tsem)
                    ln_block(hb_t[:, :, o:o + 512], hbb, 512, lambda c: ng[:, 4, c:c + 1], None, None, None, 0, rs, f32hook=hookf)
        P.touch(hb_t, hbb)
        P.release()

    for l in range(n_layers):
        ada_layer(l)
        phase_proj(l)
        phase_attn(l)
        phase_dn(l)
        phase_merge(l)
        phase_ffn(l, final=(l == n_layers - 1))
    P.emit()
    return nc


def _rope_partner_cols():
    idx = np.zeros(512, np.int64)
    for h in range(4):
        for m in range(2):
            for d in range(64):
                half = d // 32
                dd = d % 32
                pd = dd + 16 if dd < 16 else dd - 16
                idx[h * 128 + m * 64 + d] = h * 128 + m * 64 + half * 32 + pd
    return idx


def _rope_tables():
    n_freq = 16
    inv = (10000.0 ** (-np.arange(n_freq, dtype=np.float32) / n_freq)).astype(np.float32)
    t = np.arange(TL)
    row = (t // 64).astype(np.float32)
    col = (t % 64).astype(np.float32)
    ar = row[:, None] * inv
    ac = col[:, None] * inv
    C = np.ones((128, T), np.float32)
    S = np.zeros((128, T), np.float32)
    for p in range(128):
        d = p % 64
        half = d // 32
        dd = d % 32
        f = dd % 16
        ang = (ar if half == 0 else ac)[:, f]
        C[p, TC:] = np.cos(ang)
        S[p, TC:] = (-np.sin(ang)) if dd < 16 else np.sin(ang)
    return C, S


def _pool_inv():
    out = np.zeros((2, 128, 4376), np.float32)
    wins = (2, 4, 8, 16)
    for g, w in enumerate(wins):
        for (L, off) in ((TC, 8), (TL, 272)):
            t = np.arange(L)
            lo = np.clip(t - w // 2, 0, L)
            hi = np.clip(t - w // 2 + w, 0, L)
            out[g // 2, (g % 2) * 64:(g % 2 + 1) * 64, off:off + L] = (1.0 / (hi - lo).astype(np.float32))[None, :]
    return out


def _masks():
    i = np.arange(64)
    m = np.zeros((64, 6, 64), np.float32)
    NEG = -30000.0
    m[:, 0, :] = (i[:, None] <= i[None, :])
    m[:, 1, :] = (i[:, None] >= i[None, :])
    r = i[:, None]; c = i[None, :]
    m[:, 2, :] = np.where(r < c, NEG, 0.0)
    m[:, 3, :] = np.where(r <= c, NEG, 0.0)
    m[:, 4, :] = np.where(r > c, NEG, 0.0)
    m[:, 5, :] = np.where(r >= c, NEG, 0.0)
    return m


def make_inputs(b, inp):
    f = np.float32
    A = np.ascontiguousarray
    d = {}
    full = np.concatenate([inp['ctx'][b], inp['x'][b]], axis=0)
    d['xT'] = A(full.T.reshape(8, 128, T))
    cin = np.stack([inp['c'][b], inp['c_ctx']], axis=-1)
    d['cin'] = A(cin.reshape(8, 128, 2).transpose(1, 0, 2))
    d['ada_w'] = inp['ada_w'].reshape(2, 8, 128, 6144)
    d['adab'] = A(inp['ada_b'].reshape(2, 48, 128).transpose(0, 2, 1))
    ngs = np.stack([inp['norm1_g'][0], inp['norm1_g'][1], inp['norm2_g'][0], inp['norm2_g'][1], inp['final_norm_g']])
    d['ngs'] = A(ngs.reshape(5, 8, 128).transpose(2, 0, 1))
    d['w_in'] = inp['w_in'].reshape(2, 8, 128, INW)
    pidx = _rope_partner_cols()
    cols = np.concatenate([pidx, 512 + pidx])
    d['w_inp'] = A(inp['w_in'][:, :, cols]).reshape(2, 8, 128, 1024)
    C, S = _rope_tables()
    d['ropeC'] = C
    d['ropeS'] = S
    d['lam_in'] = inp['attn_lambda'].reshape(2, 256)
    sg = np.zeros((128, 2), f)
    for l in range(2):
        li = 0.8 - 0.6 * math.exp(-0.3 * l)
        sg[:, l] = inp['attn_subln_g'][l] * np.float32(1 - li)
    d['sublng'] = sg
    d['convw'] = A(inp['dn_conv_w'].reshape(2, 5, 6, 128).transpose(0, 3, 2, 1))
    d['dnc'] = A(np.concatenate([inp['dn_dt_bias'].reshape(2, 8), inp['dn_a_log'].reshape(2, 8)], axis=1))
    d['dnng'] = A(np.tile(inp['dn_norm_g'], (1, 4)))
    d['pinv'] = _pool_inv()
    pw = np.zeros((2, 2, 128, 128), f)
    for l in range(2):
        for g in range(4):
            pw[l, g // 2, (g % 2) * 64:(g % 2 + 1) * 64, (g % 2) * 64:(g % 2 + 1) * 64] = inp['pool_w'][l, g]
    d['poolw'] = pw
    d['pools'] = A(inp['pool_scale'].reshape(2, 2, 128).transpose(2, 0, 1))
    d['w_br'] = inp['w_branch'].reshape(2, 8, 128, 1024)
    d['w_out'] = inp['w_out'].reshape(2, 8, 128, 1024)
    d['fw1'] = inp['ffn_w1'].reshape(8, 128, 2816)
    d['fw3'] = inp['ffn_w3'].reshape(8, 128, 2816)
    d['fw2'] = inp['ffn_w2'].reshape(22, 128, 1024)
    d['rw'] = A(inp['router_w'][0].reshape(8, 128, 8).transpose(1, 0, 2))
    d['mw1'] = inp['moe_w1'].reshape(8, 8, 128, 3584)
    d['mw3'] = inp['moe_w3'].reshape(8, 8, 128, 3584)
    d['mw2'] = inp['moe_w2'].reshape(8, 28, 128, 1024)
    sel = np.zeros((8, 8, 128), f)
    for e in range(8):
        sel[e, e, :] = 1.0
    d['selc'] = sel
    d['masks'] = _masks()
    d['identb'] = np.eye(128, dtype=f)
    return {k: np.ascontiguousarray(v, dtype=np.float32) for k, v in d.items()}


def kernel(**inputs):
    inp = {k: np.asarray(v, dtype=np.float32) for k, v in inputs.items()}
    nc = build()
    maps = [make_inputs(b, inp) for b in range(4)]
    in_maps = [maps[i % 4] for i in range(8)]
    res = run_bass_kernel_spmd(nc, in_maps, core_ids=list(range(8)))
    out = np.zeros((4, TL, 1024), np.float32)
    for i in range(8):
        b, half = i % 4, i // 4
        y = np.asarray(res.results[i]["yT"])
        if USE_SPLIT:
            out[b, half * 2048:(half + 1) * 2048] = y.reshape(1024, 2048).T
        elif half == 0:
            out[b] = y.reshape(1024, TL).T
    return out
```

```python
import math
import numpy as np
from contextlib import ExitStack
import concourse.bass as bass
import concourse.mybir as mybir
from concourse.bass_utils import run_bass_kernel_spmd

F32 = mybir.dt.float32
BF16 = mybir.dt.bfloat16
AF = mybir.ActivationFunctionType
ALU = mybir.AluOpType
AX = mybir.AxisListType
ENGS = ('pe', 'act', 'dve', 'pool', 'sp')

USE_SPLIT = True
TC = 256
TL = 4096
T = TC + TL
BLKS = [(0, 256)] + [(256 + 512 * i, 512) for i in range(8)]
EPS = 1e-6
INW = 5904
NCH = T // 64


class Buf:
    __slots__ = ('w', 'rs')

    def __init__(self):
        self.w = None
        self.rs = {}


class Ins:
    __slots__ = ('eng', 'fn', 'deps', 'sig', 'seq', 'dma', 'dsem', 'dneed', 'idx')

    def __init__(self, eng, fn, dma=False):
        self.eng = eng
        self.fn = fn
        self.deps = {}
        self.dneed = {}
        self.sig = False
        self.seq = 0
        self.dma = dma
        self.dsem = None


class Prog:
    def __init__(self, nc):
        self.nc = nc
        self.ins = {e: [] for e in ENGS}
        self.dma_cnt = {}
        self.next_dsem = 0
        self.sb_off = 16512
        self.sb_stack = []
        self.free_dsems = []
        self.offcache = {}
        self.cur_rings = []
        self.bar_e = {}
        self.bar_d = {}
        self.cur_dsems = []
        self.nalloc = 0
        self.SB_LIMIT = 229312
        self.psums = [nc.alloc_psum_tensor(f"psb{i}", [128, 512], F32) for i in range(8)]
        self.psbufs = [Buf() for _ in range(8)]
        self.psi = 0
        self.bufs = {}

    def sb(self, shape, dtype, name=None):
        esz = 2 if dtype == BF16 else 4
        n = int(np.prod(shape[1:])) * esz
        n = (n + 63) // 64 * 64
        off = self.sb_off
        assert off + n <= self.SB_LIMIT, f"SBUF overflow {off}+{n} ({name})"
        self.sb_off += n
        self.nalloc += 1
        return self.nc.alloc_sbuf_tensor_at(f"{name or 't'}_{self.nalloc}", list(shape), dtype, offset=off)

    def mark(self):
        self.sb_stack.append((self.sb_off, self.cur_dsems, self.cur_rings))
        self.cur_dsems = []
        self.cur_rings = []

    def touch(self, t, b):
        idx = tuple(slice(0, 1) for _ in t.shape)
        self.dve(lambda e: e.memset(t[idx], 0.0), w=[b])

    def release(self):
        for rg in self.cur_rings:
            for t, b in zip(rg.t, rg.b):
                if b.w is not None or b.rs:
                    self.touch(t, b)
        self.free_dsems.extend(self.cur_dsems)
        self.sb_off, self.cur_dsems, self.cur_rings = self.sb_stack.pop()
        self.barrier()

    def barrier(self):
        for e in ENGS:
            for i in reversed(self.ins[e]):
                if not i.dma:
                    self.bar_e[e] = i
                    i.sig = True
                    break
        self.bar_d = {}

    def new_dsem(self):
        if self.free_dsems:
            i = self.free_dsems.pop()
        else:
            i = self.next_dsem
            self.next_dsem += 1
            self.dma_cnt[i] = 0
        self.cur_dsems.append(i)
        return i

    def ps(self):
        k = self.psi % 8
        self.psi += 1
        return self.psums[k], self.psbufs[k]

    def B(self, key):
        b = self.bufs.get(key)
        if b is None:
            b = self.bufs[key] = Buf()
        return b

    def _add_dep(self, ins, d, raw):
        if d is None or d is ins:
            return
        if d.dma:
            ins.dneed[d.dsem] = self.dma_cnt[d.dsem]
            return
        if d.eng == ins.eng and not raw and not ins.dma and d.eng != 'pool':
            return
        cur = ins.deps.get(d.eng)
        if cur is None or d.idx > cur.idx:
            ins.deps[d.eng] = d
        d.sig = True

    def op(self, eng, fn, r=(), w=(), dma=False, dsem=None):
        ins = Ins(eng, fn, dma)
        ins.idx = len(self.ins[eng])
        if dma:
            ins.dsem = dsem
        for b in r:
            self._add_dep(ins, b.w, True)
        for b in w:
            self._add_dep(ins, b.w, False)
            for rr in b.rs.values():
                self._add_dep(ins, rr, False)
        for e2, d in self.bar_e.items():
            if e2 != eng or dma:
                cur = ins.deps.get(e2)
                if cur is None or d.idx > cur.idx:
                    ins.deps[e2] = d
        for k, cnt in self.bar_d.items():
            if cnt > ins.dneed.get(k, 0):
                ins.dneed[k] = cnt
        if dma:
            self.dma_cnt[dsem] += 16
        key = ('d', dsem) if dma else eng
        for b in r:
            b.rs[key] = ins
        for b in w:
            b.w = ins
            b.rs = {}
        self.ins[eng].append(ins)
        return ins

    def pe(self, fn, r=(), w=()):
        return self.op('pe', fn, r, w)

    def act(self, fn, r=(), w=()):
        return self.op('act', fn, r, w)

    def dve(self, fn, r=(), w=()):
        return self.op('dve', fn, r, w)

    def pool(self, fn, r=(), w=()):
        return self.op('pool', fn, r, w)

    def dma(self, out, in_, r=(), w=(), sem=None, q='sp'):
        return self.op(q, lambda e: e.dma_start(out=out, in_=in_), r, w, dma=True, dsem=sem)

    def dmad(self, out, in_fn, r=(), w=(), sem=None, q='sp'):
        def f(e):
            off = self.offcache.get(id(e))
            if off is None:
                off = self.offcache[id(e)] = e.snap((e.partition_id() // 4) * 2048)
            return e.dma_start(out=out, in_=in_fn(off))
        return self.op(q, f, r, w, dma=True, dsem=sem)

    def emit(self):
        nc = self.nc
        for e in ENGS:
            s = 0
            for i in self.ins[e]:
                if i.sig and not i.dma:
                    s += 1
                    i.seq = s
        with ExitStack() as st:
            esem = {e: st.enter_context(nc.semaphore(f"s_{e}")) for e in ENGS if e != 'sp'}
            dsems = [st.enter_context(nc.semaphore(f"d_{k}")) for k in range(self.next_dsem)]
            block = st.enter_context(nc.Block())
            final_d = dict(self.dma_cnt)
            final_e = {e: max([i.seq for i in self.ins[e]] + [0]) for e in ENGS}

            def run(ename, eng):
                seen_e = {e: 0 for e in ENGS}
                seen_d = {}
                for i in self.ins[ename]:
                    for pe_, d in i.deps.items():
                        if d.seq > seen_e[pe_]:
                            eng.wait_ge(esem[pe_], d.seq)
                            seen_e[pe_] = d.seq
                    for k, cnt in i.dneed.items():
                        if cnt > seen_d.get(k, 0):
                            eng.wait_ge(dsems[k], cnt)
                            seen_d[k] = cnt
                    bi = i.fn(eng)
                    if i.dma:
                        bi.then_inc(dsems[i.dsem], 16)
                    elif i.sig:
                        bi.then_inc(esem[ename], 1)
                if ename == 'sp':
                    for k, cnt in final_d.items():
                        if cnt > seen_d.get(k, 0):
                            eng.wait_ge(dsems[k], cnt)
                    for e2 in ENGS:
                        if e2 != 'sp' and final_e[e2] > seen_e[e2]:
                            eng.wait_ge(esem[e2], final_e[e2])

            @block.tensor
            def _(eng):
                run('pe', eng)

            @block.scalar
            def _(eng):
                run('act', eng)

            @block.vector
            def _(eng):
                run('dve', eng)

            @block.gpsimd
            def _(eng):
                run('pool', eng)

            @block.sync
            def _(eng):
                run('sp', eng)


class Ring:
    def __init__(self, P, shape, dtype, n, name, dma=True):
        self.t = [P.sb(shape, dtype, f"{name}{i}") for i in range(n)]
        self.b = [Buf() for _ in range(n)]
        self.s = [P.new_dsem() if dma else None for _ in range(n)]
        self.i = 0
        self.n = n
        P.cur_rings.append(self)

    def next(self):
        k = self.i % self.n
        self.i += 1
        return self.t[k], self.b[k], self.s[k]


def bc(ap, shape):
    return ap.unsqueeze(len(ap.shape)).to_broadcast(list(shape))


def build(n_layers=2, debug=()):
    nc = bass.Bass("TRN2", target_bir_lowering=False)
    P = Prog(nc)
    SPLIT = (n_layers == 2) and USE_SPLIT
    TOUT = 2048 if SPLIT else TL

    def din(name, shape, dt=F32):
        return nc.dram_tensor(name, list(shape), dt, kind="ExternalInput").ap()

    def dscr(name, shape, dt=F32, out=False):
        return nc.dram_tensor(name, list(shape), dt, kind="ExternalOutput" if (out or name in debug) else "Internal").ap()

    xT = din("xT", [8, 128, T])
    cin = din("cin", [128, 8, 2])
    ada_w = din("ada_w", [2, 8, 128, 6144])
    adab = din("adab", [2, 128, 48])
    ngs = din("ngs", [128, 5, 8])
    w_in = din("w_in", [2, 8, 128, INW])
    w_inp = din("w_inp", [2, 8, 128, 1024])
    ropeC = din("ropeC", [128, T])
    ropeS = din("ropeS", [128, T])
    lam_in = din("lam_in", [2, 256])
    sublng = din("sublng", [128, 2])
    convw = din("convw", [2, 128, 6, 5])
    dnc = din("dnc", [2, 16])
    dnng = din("dnng", [2, 256])
    pinv = din("pinv", [2, 128, 4376])
    poolw = din("poolw", [2, 2, 128, 128])
    pools = din("pools", [128, 2, 2])
    w_br = din("w_br", [2, 8, 128, 1024])
    w_out = din("w_out", [2, 8, 128, 1024])
    fw1 = din("fw1", [8, 128, 2816])
    fw3 = din("fw3", [8, 128, 2816])
    fw2 = din("fw2", [22, 128, 1024])
    rw = din("rw", [128, 8, 8])
    mw1 = din("mw1", [8, 8, 128, 3584])
    mw3 = din("mw3", [8, 8, 128, 3584])
    mw2 = din("mw2", [8, 28, 128, 1024])
    selc = din("selc", [8, 8, 128])
    masks = din("masks", [64, 6, 64])
    identb = din("identb", [128, 128])
    yT = dscr("yT", [8, 128, TOUT], out=True)

    TP = T + 64
    H = dscr("H", [8, 128, TP])
    QT = dscr("QT", [4, 128, TP], BF16)
    KT = dscr("KT", [4, 128, T], BF16)
    VT = dscr("VT", [34, 128, 512], BF16)
    DNF = dscr("DNF", [4, 128, T], BF16)
    DNT = dscr("DNT", [T, 512], BF16)
    ZAB = dscr("ZAB", [T, 272])
    POOLT = dscr("POOLT", [2, 128, TP], BF16)
    GT = dscr("GT", [24, 128, TP], BF16)
    ATT = dscr("ATT", [4, 128, T], BF16)
    PREPB = dscr("PREPB", [2, NCH, 64, 1024], BF16)
    PREPF = dscr("PREPF", [2, NCH, 64, 12])
    OSC = dscr("OSC", [2, T, 256])
    DNY = dscr("DNY", [2, 128, TP], BF16)

    ones_f = P.sb([128, 128], F32, "ones_f"); onesb = Buf()
    ones_b = P.sb([128, 128], BF16, "ones_b"); ones_bb = Buf()
    blk_f = P.sb([128, 128], F32, "blk_f"); blkb = Buf()
    epsc = P.sb([128, 1], F32, "epsc"); epsb = Buf()
    idb = P.sb([128, 128], BF16, "idb"); idbb = Buf(); ids = P.new_dsem()
    idf = P.sb([128, 128], F32, "idf"); idfb = Buf()
    ng = P.sb([128, 5, 8], F32, "ng"); ngb = Buf(); ngsem = P.new_dsem()
    csil = P.sb([128, 8, 2], F32, "csil"); csb = Buf(); cssem = P.new_dsem()
    mod = P.sb([128, 2, 48], F32, "mod"); modb = Buf()
    gain = P.sb([128, 2, 2, 8], F32, "gain"); gainb = Buf()
    adb = P.sb([128, 48], F32, "adb"); adbb = Buf(); adsem = P.new_dsem()
    P.pool(lambda e: e.memset(ones_f[:], 1.0), w=[onesb])
    P.pool(lambda e: e.memset(ones_b[:], 1.0), w=[ones_bb])
    P.pool(lambda e: e.memset(blk_f[:], 0.0), w=[blkb])
    P.pool(lambda e: e.memset(blk_f[0:64, 0:64], 1.0), w=[blkb])
    P.pool(lambda e: e.memset(blk_f[64:128, 64:128], 1.0), w=[blkb])
    P.pool(lambda e: e.memset(epsc[:], EPS), w=[epsb])
    P.dma(idb[:], identb, w=[idbb], sem=ids, q='pool')
    P.dma(idf[:], identb, w=[idfb], sem=ids)
    P.dma(ng[:], ngs, w=[ngb], sem=ngsem)
    P.dma(csil[:], cin, w=[csb], sem=cssem)
    P.act(lambda e: e.activation(out=csil[:], in_=csil[:], func=AF.Silu), r=[csb], w=[csb])

    def dbg_dump(name, ap_dram):
        pass

    def ada_layer(l):
        P.mark()
        aw = Ring(P, [128, 8, 1024], F32, 2, "aw")
        P.dma(adb[:], adab[l], w=[adbb], sem=adsem)
        ps, pb = P.ps()
        for j in range(6):
            t, b, s = aw.next()
            for k in range(8):
                P.dma(t[:, k, :], ada_w[l, k, :, j * 1024:(j + 1) * 1024], w=[b], sem=s)
            for c in range(8):
                col = (j * 8 + c) * 2
                for k in range(8):
                    P.pe(lambda e, t=t, k=k, c=c, col=col: e.matmul(ps[:, col:col + 2], lhsT=t[:, k, c * 128:(c + 1) * 128],
                                                                    rhs=csil[:, k, :], start=(k == 0), stop=(k == 7)),
                         r=[b, csb], w=[pb])
        psv = ps[:, 0:96].rearrange("p (a s) -> p a s", s=2)
        for s_ in range(2):
            P.dve(lambda e, s_=s_: e.tensor_tensor(out=mod[:, s_, :], in0=psv[:, :, s_], in1=adb[:], op=ALU.add),
                  r=[pb, adbb], w=[modb])
            P.dve(lambda e, s_=s_: e.scalar_tensor_tensor(out=gain[:, 0, s_, :], in0=mod[:, s_, 8:16], scalar=1.0,
                                                          in1=ng[:, l, :], op0=ALU.add, op1=ALU.mult),
                  r=[modb, ngb], w=[gainb])
            P.dve(lambda e, s_=s_: e.scalar_tensor_tensor(out=gain[:, 1, s_, :], in0=mod[:, s_, 32:40], scalar=1.0,
                                                          in1=ng[:, 2 + l, :], op0=ALU.add, op1=ALU.mult),
                  r=[modb, ngb], w=[gainb])
        P.release()

    def ln_block(hsrc, hb_, n, gcol, scol, out_bf, outb, t0o, rs, f32hook=None):
        sq, sqb, _ = rs['sq'].next()
        P.act(lambda e: e.activation(out=sq[:, :, :n], in_=hsrc[:, :, :n], func=AF.Square), r=[hb_], w=[sqb])
        ps, pb = P.ps()
        for c in range(8):
            P.pe(lambda e, c=c: e.matmul(ps[:, :n], lhsT=ones_f[:], rhs=sq[:, c, :n], start=(c == 0), stop=(c == 7)),
                 r=[onesb, sqb], w=[pb])
        rstd, rb, _ = rs['rstd'].next()
        P.act(lambda e: e.activation(out=rstd[:, :n], in_=ps[:, :n], func=AF.Sqrt, bias=epsc[:], scale=1.0 / 1024),
              r=[pb, epsb], w=[rb])
        P.dve(lambda e: e.reciprocal(out=rstd[:, :n], in_=rstd[:, :n]), r=[rb], w=[rb])
        for c in range(8):
            tmp, tb, tsem = rs['tmp'].next()
            P.dve(lambda e, c=c, tmp=tmp: e.scalar_tensor_tensor(out=tmp[:, :n], in0=hsrc[:, c, :n], scalar=gcol(c),
                                                                 in1=rstd[:, :n], op0=ALU.mult, op1=ALU.mult),
                  r=[hb_, gainb, rb, ngb], w=[tb])
            if scol is None:
                f32hook(c, tmp, tb, tsem)
                continue
            if f32hook is None:
                P.act(lambda e, c=c, tmp=tmp: e.activation(out=out_bf[:, c, t0o:t0o + n], in_=tmp[:, :n], func=AF.Identity,
                                                           bias=scol(c), scale=1.0), r=[tb, modb], w=[outb])
            else:
                P.act(lambda e, c=c, tmp=tmp: e.activation(out=tmp[:, :n], in_=tmp[:, :n], func=AF.Identity,
                                                           bias=scol(c), scale=1.0), r=[tb, modb], w=[tb])
                P.pool(lambda e, c=c, tmp=tmp: e.tensor_copy(out=out_bf[:, c, t0o:t0o + n], in_=tmp[:, :n]), r=[tb], w=[outb])
                f32hook(c, tmp, tb, tsem)

    def ln_rings():
        return {'sq': Ring(P, [128, 8, 512], F32, 1, "sq"), 'rstd': Ring(P, [128, 512], F32, 2, "rstd"),
                'tmp': Ring(P, [128, 512], F32, 3, "tmp")}

    def hsrc_of(l):
        return xT if l == 0 else H

    def phase_proj(l):
        ctx_out = l == 0
        P.mark()
        uT = P.sb([128, 8, T], BF16, "uT"); ub = Buf()
        P.mark()
        rs = ln_rings()
        hr = Ring(P, [128, 8, 512], F32, 2, "hblk")
        src = hsrc_of(l)
        for (t0, n) in BLKS:
            s_ = 1 if t0 == 0 else 0
            h, hb_, hs = hr.next()
            P.dma(h[:, :, :n], src[:, :, t0:t0 + n].rearrange("c p t -> p c t"), r=[P.B(('H', t0))], w=[hb_], sem=hs)
            ln_block(h, hb_, n, lambda c, s_=s_: gain[:, 0, s_, c:c + 1], lambda c, s_=s_: mod[:, s_, c:c + 1],
                     uT, ub, t0, rs)
        P.release()

        wrs = {512: Ring(P, [128, 8, 512], BF16, 2, "wt512"), 272: Ring(P, [128, 8, 272], BF16, 1, "wt272"),
               128: Ring(P, [128, 8, 128], BF16, 4, "wt128")}
        wl = w_in[l]

        def loadw(c0, w, src_=None):
            t, b, s = wrs[w].next()
            P.dma(t[:], (src_ if src_ is not None else wl)[:, :, c0:c0 + w].rearrange("c p n -> p c n"),
                  w=[b], sem=s, q='pool')
            return t, b

        def gemm_fm(wt, wb_, j0, t0, n):
            ps, pb = P.ps()
            for c in range(8):
                P.pe(lambda e, c=c: e.matmul(ps[:, :n], lhsT=wt[:, c, j0:j0 + 128], rhs=uT[:, c, t0:t0 + n],
                                             start=(c == 0), stop=(c == 7)), r=[wb_, ub], w=[pb])
            return ps, pb

        P.mark()
        rc = P.sb([128, T], F32, "ropeC"); rcb = Buf(); rsm = P.new_dsem()
        rsn = P.sb([128, T], F32, "ropeS"); rsb = Buf()
        P.dma(rc[:], ropeC, w=[rcb], sem=rsm)
        P.dma(rsn[:], ropeS, w=[rsb], sem=rsm)
        t1r = Ring(P, [128, 512], F32, 2, "t1")
        t2r = Ring(P, [128, 512], F32, 2, "t2")
        obr = Ring(P, [128, T], BF16, 2, "qkout")
        for nm, col0, dst in (('k', 512, KT), ('q', 0, QT)):
            for hh in range(4):
                wt, wb_ = loadw(col0 + hh * 128, 128)
                wt2, wb2 = loadw(col0 + hh * 128, 128, w_inp[l])
                ob, obb, obs = obr.next()
                for (t0, n) in BLKS:
                    if nm == 'q' and t0 == 0 and not ctx_out:
                        continue
                    ps1, pb1 = gemm_fm(wt, wb_, 0, t0, n)
                    ps2, pb2 = gemm_fm(wt2, wb2, 0, t0, n)
                    t1, t1b, _ = t1r.next()
                    t2, t2b, _ = t2r.next()
                    P.dve(lambda e, ps1=ps1, t1=t1, t0=t0, n=n: e.tensor_tensor(out=t1[:, :n], in0=ps1[:, :n], in1=rc[:, t0:t0 + n], op=ALU.mult),
                          r=[pb1, rcb], w=[t1b])
                    P.dve(lambda e, ps2=ps2, t2=t2, t0=t0, n=n: e.tensor_tensor(out=t2[:, :n], in0=ps2[:, :n], in1=rsn[:, t0:t0 + n], op=ALU.mult),
                          r=[pb2, rsb], w=[t2b])
                    P.pool(lambda e, t1=t1, t2=t2, ob=ob, t0=t0, n=n: e.tensor_tensor(out=ob[:, t0:t0 + n], in0=t1[:, :n], in1=t2[:, :n], op=ALU.add),
                           r=[t1b, t2b], w=[obb])
                lo = 0 if (nm == 'k' or ctx_out) else 256
                P.dma(dst[hh, :, lo:T], ob[:, lo:T], r=[obb], w=[P.B((nm, hh))], sem=obs)
        P.release()

        P.mark()
        vst = Ring(P, [128, 512], BF16, 3, "vst")
        wt, wb_ = loadw(1024, 512)
        for tt in range(34):
            ps, pb = P.ps()
            for c in range(8):
                P.pe(lambda e, c=c, ps=ps, tt=tt, wt=wt: e.matmul(ps[:, :], lhsT=uT[:, c, tt * 128:(tt + 1) * 128], rhs=wt[:, c, :512],
                                                           start=(c == 0), stop=(c == 7)), r=[wb_, ub], w=[pb])
            st, sb_, ss = vst.next()
            P.act(lambda e, ps=ps, st=st: e.copy(out=st[:], in_=ps[:]), r=[pb], w=[sb_])
            P.dma(VT[tt], st[:], r=[sb_], w=[P.B(('V', tt))], sem=ss)
        zst = Ring(P, [128, 272], F32, 3, "zst")
        dcn = P.sb([128, 16], F32, "dcn"); dcb = Buf(); dcs = P.new_dsem()
        P.dma(dcn[:], dnc[l].partition_broadcast(128), w=[dcb], sem=dcs)
        P.act(lambda e: e.activation(out=dcn[:, 8:16], in_=dcn[:, 8:16], func=AF.Exp), r=[dcb], w=[dcb])
        P.dve(lambda e: e.tensor_scalar(out=dcn[:, 8:16], in0=dcn[:, 8:16], scalar1=-1.0, scalar2=None, op0=ALU.mult), r=[dcb], w=[dcb])
        wt, wb_ = loadw(2304, 272)
        for tt in range(34):
            ps, pb = P.ps()
            for c in range(8):
                P.pe(lambda e, c=c, ps=ps, tt=tt, wt=wt: e.matmul(ps[:, :272], lhsT=uT[:, c, tt * 128:(tt + 1) * 128], rhs=wt[:, c, :272],
                                                           start=(c == 0), stop=(c == 7)), r=[wb_, ub], w=[pb])
            st, sb_, ss = zst.next()
            P.act(lambda e, ps=ps, st=st: e.activation(out=st[:, 0:256], in_=ps[:, 0:256], func=AF.Silu), r=[pb], w=[sb_])
            P.dve(lambda e, ps=ps, st=st: e.tensor_tensor(out=st[:, 256:264], in0=ps[:, 256:264], in1=dcn[:, 0:8], op=ALU.add), r=[pb, dcb], w=[sb_])
            P.act(lambda e, st=st: e.activation(out=st[:, 256:264], in_=st[:, 256:264], func=AF.Exp), r=[sb_], w=[sb_])
            P.act(lambda e, st=st: e.activation(out=st[:, 256:264], in_=st[:, 256:264], func=AF.Ln, bias=1.0, scale=1.0), r=[sb_], w=[sb_])
            P.dve(lambda e, st=st: e.tensor_tensor(out=st[:, 256:264], in0=st[:, 256:264], in1=dcn[:, 8:16], op=ALU.mult), r=[sb_, dcb], w=[sb_])
            P.act(lambda e, ps=ps, st=st: e.activation(out=st[:, 264:272], in_=ps[:, 264:272], func=AF.Sigmoid), r=[pb], w=[sb_])
            P.dma(ZAB[tt * 128:(tt + 1) * 128, :], st[:], r=[sb_], w=[P.B(('Z', tt))], sem=ss)
        P.release()

        P.mark()
        gst = Ring(P, [128, 512], BF16, 4, "gst")
        for g4 in range(6):
            wt, wb_ = loadw(2832 + g4 * 512, 512)
            for j in range(4):
                for (t0, n) in BLKS:
                    if t0 == 0 and not ctx_out:
                        continue
                    ps, pb = gemm_fm(wt, wb_, j * 128, t0, n)
                    st, sb_, ss = gst.next()
                    P.act(lambda e, ps=ps, st=st, n=n: e.activation(out=st[:, :n], in_=ps[:, :n], func=AF.Sigmoid), r=[pb], w=[sb_])
                    P.dma(GT[g4 * 4 + j, :, t0:t0 + n], st[:, :n], r=[sb_], w=[P.B(('G', g4 * 4 + j, t0))], sem=ss)
        P.release()

        P.mark()
        raw = P.sb([128, 4358], F32, "raw"); rawb = Buf()
        acc = P.sb([128, 4358], F32, "acc"); accb = Buf()
        sq2 = P.sb([128, 4358], F32, "sq2"); sq2b = Buf()
        nrm = Ring(P, [128, 4358], BF16, 2, "nrm")
        cw = P.sb([128, 6, 5], F32, "cw"); cwb = Buf(); cws = P.new_dsem()
        rsr = Ring(P, [128, 512], F32, 2, "rs2")
        tst = Ring(P, [128, 128], BF16, 3, "tst")
        P.dma(cw[:], convw[l], w=[cwb], sem=cws)
        P.pool(lambda e: e.memset(raw[:], 0.0), w=[rawb])
        NW = 4354
        for i in range(6):
            wt, wb_ = loadw(1536 + i * 128, 128)
            for (t0, n) in BLKS:
                o = t0 + 2 if t0 == 0 else t0 + 4
                ps, pb = gemm_fm(wt, wb_, 0, t0, n)
                P.act(lambda e, ps=ps, o=o, n=n: e.copy(out=raw[:, o:o + n], in_=ps[:, :n]), r=[pb], w=[rawb])
            P.dve(lambda e, i=i: e.tensor_scalar(out=acc[:, 2:2 + NW], in0=raw[:, 0:NW], scalar1=cw[:, i, 0:1], scalar2=None, op0=ALU.mult),
                  r=[rawb, cwb], w=[accb])
            for k in range(1, 5):
                eng = P.dve
                eng(lambda e, i=i, k=k: e.scalar_tensor_tensor(out=acc[:, 2:2 + NW], in0=raw[:, k:k + NW], scalar=cw[:, i, k:k + 1],
                                                               in1=acc[:, 2:2 + NW], op0=ALU.mult, op1=ALU.add),
                    r=[rawb, cwb, accb], w=[accb])
            P.act(lambda e: e.activation(out=acc[:, 2:2 + NW], in_=acc[:, 2:2 + NW], func=AF.Silu), r=[accb], w=[accb])
            nr, nrb, nrs = nrm.next()
            if i < 4:
                P.pool(lambda e: e.tensor_tensor(out=sq2[:, 2:2 + NW], in0=acc[:, 2:2 + NW], in1=acc[:, 2:2 + NW], op=ALU.mult),
                       r=[accb], w=[sq2b])
                for (t0, n) in BLKS:
                    o = t0 + 2 if t0 == 0 else t0 + 4
                    ps, pb = P.ps()
                    P.pe(lambda e, ps=ps, o=o, n=n: e.matmul(ps[:, :n], lhsT=blk_f[:], rhs=sq2[:, o:o + n], start=True, stop=True),
                         r=[blkb, sq2b], w=[pb])
                    r2, r2b, _ = rsr.next()
                    P.act(lambda e, ps=ps, r2=r2, n=n: e.activation(out=r2[:, :n], in_=ps[:, :n], func=AF.Sqrt, bias=epsc[:], scale=1.0),
                          r=[pb, epsb], w=[r2b])
                    P.dve(lambda e, r2=r2, n=n: e.reciprocal(out=r2[:, :n], in_=r2[:, :n]), r=[r2b], w=[r2b])
                    P.dve(lambda e, r2=r2, nr=nr, o=o, n=n, i=i: e.scalar_tensor_tensor(
                        out=nr[:, o:o + n], in0=acc[:, o:o + n], scalar=(0.125 if i < 2 else 1.0), in1=r2[:, :n],
                        op0=ALU.mult, op1=ALU.mult), r=[accb, r2b], w=[nrb])
                P.dma(DNF[i, :, 0:256], nr[:, 2:258], r=[nrb], w=[P.B(('DNF', i))], sem=nrs)
                P.dma(DNF[i, :, 256:T], nr[:, 260:4356], r=[nrb], w=[P.B(('DNF', i))], sem=nrs)
            else:
                P.pool(lambda e, nr=nr: e.tensor_copy(out=nr[:, 2:2 + NW], in_=acc[:, 2:2 + NW]), r=[accb], w=[nrb])
            if i >= 2:
                for tt in range(34):
                    o = tt * 128 + 2 if tt < 2 else tt * 128 + 4
                    ps, pb = P.ps()
                    psv = ps[:, 0:64].bitcast(BF16)
                    P.pe(lambda e, psv=psv, nr=nr, o=o: e.transpose(out=psv, in_=nr[:, o:o + 128], identity=idb[:]),
                         r=[nrb, idbb], w=[pb])
                    st, sb_, ss = tst.next()
                    P.act(lambda e, psv=psv, st=st: e.copy(out=st[:], in_=psv), r=[pb], w=[sb_])
                    P.dma(DNT[tt * 128:(tt + 1) * 128, (i - 2) * 128:(i - 1) * 128], st[:], r=[sb_], w=[P.B(('DNT', tt, i))], sem=ss)
        P.release()

        P.mark()
        praw = P.sb([128, 4376], F32, "praw"); prb = Buf()
        pa = P.sb([128, 4376], F32, "pa"); pab = Buf()
        pbt = P.sb([128, 4376], F32, "pbt"); pbb = Buf()
        pv = P.sb([128, 4376], F32, "pv"); pvb = Buf(); pvs = P.new_dsem()
        pw = P.sb([128, 128], F32, "pw"); pwb = Buf(); pws = P.new_dsem()
        psc = P.sb([128, 2, 2], F32, "psc"); pscb = Buf()
        pst = Ring(P, [128, T], BF16, 1, "pst")
        P.dma(psc[:], pools, w=[pscb], sem=pws)
        P.pool(lambda e: e.memset(praw[:], 0.0), w=[prb])
        for i in range(2):
            wt, wb_ = loadw(2576 + i * 128, 128)
            P.dma(pv[:], pinv[i], w=[pvb], sem=pvs)
            P.dma(pw[:], poolw[l, i], w=[pwb], sem=pws)
            for (t0, n) in BLKS:
                o = t0 + 8 if t0 == 0 else t0 + 16
                ps, pb = gemm_fm(wt, wb_, 0, t0, n)
                P.act(lambda e, ps=ps, o=o, n=n: e.copy(out=praw[:, o:o + n], in_=ps[:, :n]), r=[pb], w=[prb])
            P.dve(lambda e: e.tensor_tensor(out=pa[:, 1:4376], in0=praw[:, 0:4375], in1=praw[:, 1:4376], op=ALU.add), r=[prb], w=[pab])
            P.pool(lambda e: e.memset(pa[:, 0:1], 0.0), w=[pab])
            P.pool(lambda e: e.memset(pbt[:, 0:8], 0.0), w=[pbb])
            P.pool(lambda e: e.memset(pbt[:, 4368:4376], 0.0), w=[pbb])
            P.dve(lambda e: e.tensor_tensor(out=pbt[:, 1:4375], in0=pa[:, 0:4374], in1=pa[:, 2:4376], op=ALU.add), r=[pab], w=[pbb])
            if i == 0:
                lo_src, lob, hi_src, hib = pa, pab, pbt, pbb
            else:
                P.pool(lambda e: e.memset(pa[:, 0:8], 0.0), w=[pab])
                P.pool(lambda e: e.memset(pa[:, 4368:4376], 0.0), w=[pab])
                P.dve(lambda e: e.tensor_tensor(out=pa[:, 2:4374], in0=pbt[:, 0:4372], in1=pbt[:, 4:4376], op=ALU.add), r=[pbb], w=[pab])
                P.dve(lambda e: e.tensor_tensor(out=pbt[:, 4:4372], in0=pa[:, 0:4368], in1=pa[:, 8:4376], op=ALU.add), r=[pab], w=[pbb])
                lo_src, lob, hi_src, hib = pa, pab, pbt, pbb
            P.dve(lambda e, s_=lo_src: e.tensor_tensor(out=s_[0:64, 8:4368], in0=s_[0:64, 8:4368], in1=pv[0:64, 8:4368], op=ALU.mult), r=[lob, pvb], w=[lob])
            P.pool(lambda e, s_=lo_src: e.tensor_tensor(out=s_[0:64, 8:4368], in0=s_[0:64, 8:4368], in1=praw[0:64, 8:4368], op=ALU.subtract), r=[lob, prb], w=[lob])
            P.dve(lambda e, s_=hi_src: e.tensor_tensor(out=s_[64:128, 8:4368], in0=s_[64:128, 8:4368], in1=pv[64:128, 8:4368], op=ALU.mult), r=[hib, pvb], w=[hib])
            P.pool(lambda e, s_=hi_src, d_=lo_src: e.tensor_tensor(out=d_[64:128, 8:4368], in0=s_[64:128, 8:4368], in1=praw[64:128, 8:4368], op=ALU.subtract), r=[hib, prb, lob], w=[lob])
            st, sb_, ss = pst.next()
            for (t0, n) in BLKS:
                o = t0 + 8 if t0 == 0 else t0 + 16
                ps, pb = P.ps()
                P.pe(lambda e, ps=ps, o=o, n=n, s_=lo_src: e.matmul(ps[:, :n], lhsT=pw[:], rhs=s_[:, o:o + n], start=True, stop=True),
                     r=[pwb, lob], w=[pb])
                P.act(lambda e, ps=ps, st=st, t0=t0, n=n, i=i: e.activation(out=st[:, t0:t0 + n], in_=ps[:, :n], func=AF.Copy, scale=psc[:, l, i:i + 1]),
                      r=[pb, pscb], w=[sb_])
            P.dma(POOLT[i, :, 0:T], st[:], r=[sb_], w=[P.B(('POOL', i))], sem=ss)
        P.release()
        P.release()

    def phase_attn(l):
        ctx_out = l == 0
        lam_init = 0.8 - 0.6 * math.exp(-0.3 * l)
        P.mark()
        lt = P.sb([128, 4, 64], F32, "lt"); ltb = Buf(); lts = P.new_dsem()
        l2 = P.sb([128, 2, 64], F32, "l2"); l2b = Buf()
        lr = P.sb([128, 2], F32, "lr"); lrb = Buf()
        nlam = P.sb([128, 1], F32, "nlam"); nlb = Buf()
        sg = P.sb([128, 2], F32, "sg"); sgb = Buf()
        eps128 = P.sb([128, 1], F32, "e128"); e128b = Buf()
        P.dma(lt[:], lam_in[l].rearrange("(a b) -> a b", a=4).partition_broadcast(128), w=[ltb], sem=lts)
        P.dma(sg[:], sublng, w=[sgb], sem=lts)
        ltv = lt[:].rearrange("p (x y) d -> p x y d", y=2)
        P.dve(lambda e: e.tensor_tensor(out=l2[:], in0=ltv[:, :, 0, :], in1=ltv[:, :, 1, :], op=ALU.mult), r=[ltb], w=[l2b])
        P.dve(lambda e: e.tensor_reduce(out=lr[:], in_=l2[:], axis=AX.X, op=ALU.add), r=[l2b], w=[lrb])
        P.act(lambda e: e.activation(out=lr[:], in_=lr[:], func=AF.Exp), r=[lrb], w=[lrb])
        P.dve(lambda e: e.scalar_tensor_tensor(out=nlam[:], in0=lr[:, 1:2], scalar=-lam_init, in1=lr[:, 0:1], op0=ALU.add, op1=ALU.subtract),
              r=[lrb], w=[nlb])
        P.pool(lambda e: e.memset(eps128[:], EPS), w=[e128b])
        kt = P.sb([128, T], BF16, "kt"); ktb = Buf(); kts = P.new_dsem()
        qt = P.sb([128, T], BF16, "qt"); qtb = Buf(); qts = P.new_dsem()
        vt = P.sb([128, 34, 128], BF16, "vt"); vtb = Buf(); vts = P.new_dsem()
        pr = Ring(P, [128, 512], BF16, 4, "pT")
        rr = Ring(P, [128, 512], F32, 2, "rr")
        tr = Ring(P, [128, 512], F32, 3, "tr")
        ost = Ring(P, [128, 512], BF16, 2, "ost")
        lacc = [[(P.sb([128, 512], F32, f"lacc{m}{k}"), Buf()) for k in range(2)] for m in range(2)]
        own = SPLIT and l == 1

        def attn_block(hh, t0, n):
            nkt = 2 if (t0 == 0 and not own) else 34
            accs = [P.ps() for _ in range(4)]
            sbanks = [P.ps() for _ in range(4)]
            iters = [(m, ki) for m in range(2) for ki in range(nkt)]
            for m in range(2):
                for k in range(2):
                    la, lab = lacc[m][k]
                    P.pool(lambda e, la=la: e.memset(la[:], 0.0), w=[lab])
            wbk, wbkb = sbanks[3]
            for _ in range(14):
                P.pe(lambda e: e.matmul(wbk[:, :512], lhsT=ones_b[:], rhs=kt[:, 0:512], start=True, stop=True), r=[ones_bb, ktb], w=[wbkb])

            def issue_S(j):
                m, ki = iters[j]
                psS, psb_ = sbanks[j % 4]
                P.pe(lambda e: e.matmul(psS[:, :n], lhsT=kt[m * 64:(m + 1) * 64, ki * 128:(ki + 1) * 128],
                                        rhs=qt[m * 64:(m + 1) * 64, t0:t0 + n], start=True, stop=True),
                     r=[ktb, qtb], w=[psb_])

            def issue_rest(j):
                m, ki = iters[j]
                psS, psb_ = sbanks[j % 4]
                po, pob = accs[2 * m]
                pl_, plb = accs[2 * m + 1]
                pT, pTb, _ = pr.next()
                P.act(lambda e: e.activation(out=pT[:, :n], in_=psS[:, :n], func=AF.Exp, scale=0.125), r=[psb_], w=[pTb])
                P.pe(lambda e: e.matmul(po[:, :n], lhsT=vt[:, ki, :], rhs=pT[:, :n], start=(ki == 0), stop=(ki == nkt - 1)),
                     r=[vtb, pTb], w=[pob])
                la, lab = lacc[m][ki % 2]
                P.dve(lambda e: e.tensor_tensor(out=la[:, :n], in0=la[:, :n], in1=pT[:, :n], op=ALU.add), r=[lab, pTb], w=[lab])

            LOOK = 2
            for j in range(len(iters)):
                issue_S(j)
                if j >= LOOK:
                    issue_rest(j - LOOK)
            for j in range(max(0, len(iters) - LOOK), len(iters)):
                issue_rest(j)
            attn_tail(hh, t0, n, accs, sbanks)

        def attn_tail(hh, t0, n, accs, sbanks):
            (po0, pob0), (pl0, plb0), (po1, pob1), (pl1, plb1) = accs
            for m, (pl_, plb) in ((0, (pl0, plb0)), (1, (pl1, plb1))):
                (la0, la0b), (la1, la1b) = lacc[m]
                P.dve(lambda e, la0=la0, la1=la1: e.tensor_tensor(out=la0[:, :n], in0=la0[:, :n], in1=la1[:, :n], op=ALU.add), r=[la0b, la1b], w=[la0b])
                P.pe(lambda e, pl_=pl_, la0=la0: e.matmul(pl_[:, :n], lhsT=ones_f[:], rhs=la0[:, :n], start=True, stop=True), r=[onesb, la0b], w=[plb])
            r0, r0b, _ = rr.next()
            r1, r1b, _ = rr.next()
            P.dve(lambda e, r0=r0, pl0=pl0, n=n: e.reciprocal(out=r0[:, :n], in_=pl0[:, :n]), r=[plb0], w=[r0b])
            P.dve(lambda e, r1=r1, pl1=pl1, n=n: e.reciprocal(out=r1[:, :n], in_=pl1[:, :n]), r=[plb1], w=[r1b])
            ta, tab, _ = tr.next()
            tb_, tbb, _ = tr.next()
            P.dve(lambda e, ta=ta, po0=po0, r0=r0, n=n: e.tensor_tensor(out=ta[:, :n], in0=po0[:, :n], in1=r0[:, :n], op=ALU.mult), r=[pob0, r0b], w=[tab])
            P.dve(lambda e, tb_=tb_, po1=po1, r1=r1, n=n: e.tensor_tensor(out=tb_[:, :n], in0=po1[:, :n], in1=r1[:, :n], op=ALU.mult), r=[pob1, r1b], w=[tbb])
            P.dve(lambda e, ta=ta, tb_=tb_, n=n: e.scalar_tensor_tensor(out=ta[:, :n], in0=tb_[:, :n], scalar=nlam[:, 0:1], in1=ta[:, :n], op0=ALU.mult, op1=ALU.add),
                   r=[tab, tbb, nlb], w=[tab])
            P.act(lambda e, ta=ta, tb_=tb_, n=n: e.activation(out=tb_[:, :n], in_=ta[:, :n], func=AF.Square), r=[tab], w=[tbb])
            pss, pssb = sbanks[0]
            P.pe(lambda e, pss=pss, tb_=tb_, n=n: e.matmul(pss[:, :n], lhsT=ones_f[:], rhs=tb_[:, :n], start=True, stop=True), r=[onesb, tbb], w=[pssb])
            P.act(lambda e, pss=pss, tb_=tb_, n=n: e.activation(out=tb_[:, :n], in_=pss[:, :n], func=AF.Sqrt, bias=eps128[:], scale=1.0 / 128), r=[pssb, e128b], w=[tbb])
            P.dve(lambda e, tb_=tb_, n=n: e.reciprocal(out=tb_[:, :n], in_=tb_[:, :n]), r=[tbb], w=[tbb])
            st, sb_, ss = ost.next()
            P.dve(lambda e, st=st, ta=ta, tb_=tb_, n=n: e.scalar_tensor_tensor(out=st[:, :n], in0=ta[:, :n], scalar=sg[:, l:l + 1], in1=tb_[:, :n], op0=ALU.mult, op1=ALU.mult),
                  r=[tab, tbb, sgb], w=[sb_])
            P.dma(ATT[hh, :, t0:t0 + n], st[:, :n], r=[sb_], w=[P.B(('ATT', hh, t0))], sem=ss)
        for hh in range(4):
            P.dma(kt[:], KT[hh], r=[P.B(('k', hh))], w=[ktb], sem=kts)
            P.dma(vt[:], VT[:, :, hh * 128:(hh + 1) * 128].rearrange("t p e -> p t e"), r=[P.B(('V', tt)) for tt in range(34)], w=[vtb], sem=vts)
            if own:
                P.dmad(qt[:, 256:256 + 2048], lambda off, hh=hh: QT[hh][:, bass.ds(256 + off, 2048)], r=[P.B(('q', hh))], w=[qtb], sem=qts)
                qblocks = [(256 + 512 * j, 512) for j in range(4)]
            else:
                P.dma(qt[:], QT[hh, :, 0:T], r=[P.B(('q', hh))], w=[qtb], sem=qts)
                qblocks = [b_ for b_ in BLKS if not (b_[0] == 0 and not ctx_out)]
            for (t0, n) in qblocks:
                attn_block(hh, t0, n)
        P.release()

    def phase_dn(l):
        ctx_out = l == 0
        P.mark()
        mk = P.sb([64, 6, 64], F32, "mk"); mkb = Buf(); mks = P.new_dsem()
        P.dma(mk[:], masks, w=[mkb], sem=mks)
        U = mk[:, 0, :]; L = mk[:, 1, :]
        ones64 = ones_f[0:64, 0:64]
        id64 = idf[0:64, 0:64]
        id64b = idb[0:64, 0:64]
        cfg = [dict(cum=U, m_strict=3, t_incl=4), dict(cum=L, m_strict=5, t_incl=2)]
        KP = 3

        def mk_slot(i):
            return dict(
                fqr=Ring(P, [64, 8, 64], BF16, 2, f"fq{i}"), tkr=Ring(P, [64, 512], BF16, 2, f"tok{i}"), gbr=Ring(P, [64, 16], F32, 2, f"gb{i}"),
                r0r=Ring(P, [64, 8, 64], F32, 1, f"R0{i}", dma=False), r1r=Ring(P, [64, 8, 64], F32, 1, f"R1{i}", dma=False),
                dcr=Ring(P, [64, 8, 64], F32, 1, f"dec{i}", dma=False), dtr=Ring(P, [64, 8, 64], F32, 1, f"decT{i}", dma=False),
                sc=Ring(P, [64, 32], F32, 2, f"sc{i}", dma=False), nr_=Ring(P, [64, 8, 64], BF16, 1, f"N{i}", dma=False), ntr=Ring(P, [64, 8, 64], BF16, 1, f"NT{i}", dma=False),
                p2r=Ring(P, [64, 8, 64], BF16, 2, f"P2{i}", dma=False), p2tr=Ring(P, [64, 8, 64], BF16, 2, f"P2T{i}", dma=False), ttr=Ring(P, [64, 8, 64], BF16, 2, f"TT{i}", dma=False),
                outb=Ring(P, [64, 2, 1024], BF16, 2, f"prepb{i}"), outf=Ring(P, [64, 2, 12], F32, 2, f"prepf{i}"),
                tmpf=Ring(P, [64, 8, 64], F32, 2, f"tmpf{i}", dma=False), kqr=Ring(P, [64, 4, 128], F32, 1, f"kq{i}", dma=False))
        slots = [mk_slot(i) for i in range(KP)]

        def _prep(c):
            SL = slots[c % KP]
            fq, fqb, fqs = SL['fqr'].next()
            tk, tkb, tks = SL['tkr'].next()
            gb, gbb, gbs = SL['gbr'].next()
            P.dma(fq[:], DNF.rearrange("i (h p) t -> p (i h) t", h=2)[:, :, c * 64:(c + 1) * 64], r=[P.B(('DNF', i)) for i in range(4)], w=[fqb], sem=fqs)
            P.dma(tk[:], DNT[c * 64:(c + 1) * 64, :], r=[P.B(('DNT', c // 2, i)) for i in range(2, 6)], w=[tkb], sem=tks)
            P.dma(gb[:], ZAB[c * 64:(c + 1) * 64, 256:272], r=[P.B(('Z', c // 2))], w=[gbb], sem=gbs)
            psA, psAb = P.ps()
            psAv = psA[0:64, :].rearrange("p (h x) -> p h x", x=128)
            for hh in range(4):
                P.pe(lambda e, hh=hh, fq=fq, psAv=psAv: e.matmul(psAv[:, hh, :], lhsT=fq[:, 4 + hh, :], rhs=fq[:, hh::4, :], start=True, stop=True),
                     r=[fqb], w=[psAb])
            yield
            kq, kqb, _ = SL['kqr'].next()
            P.act(lambda e, kq=kq, psA=psA: e.copy(out=kq[:].rearrange("p h x -> p (h x)"), in_=psA[0:64, :]), r=[psAb], w=[kqb])
            R0, R0b, _ = SL['r0r'].next()
            R1, R1b, _ = SL['r1r'].next()
            g8 = gb[:, 0:8]
            P.dve(lambda e, R0=R0, g8=g8: e.tensor_copy(out=R0[:], in_=bc(g8, [64, 8, 64])), r=[gbb], w=[R0b])
            for d in range(2):
                cm = cfg[d]['cum']
                P.pool(lambda e, R1=R1, d=d, cm=cm, g8=g8: e.tensor_tensor(out=R1[:, d * 4:(d + 1) * 4, :], in0=bc(g8[:, d * 4:(d + 1) * 4], [64, 4, 64]),
                                                                          in1=cm.unsqueeze(1).to_broadcast([64, 4, 64]), op=ALU.mult),
                       r=[gbb, mkb], w=[R1b])
            psD, psDb = P.ps()
            psT, psTb = P.ps()
            psDv = psD[0:64, :].rearrange("p (a x) -> p a x", x=64)
            psTv = psT[0:64, :].rearrange("p (a x) -> p a x", x=64)
            nR0, nR0b, _ = SL['tmpf'].next()
            nR1, nR1b, _ = SL['tmpf'].next()
            P.dve(lambda e, nR0=nR0, R0=R0: e.tensor_scalar(out=nR0[:], in0=R0[:], scalar1=-1.0, scalar2=None, op0=ALU.mult), r=[R0b], w=[nR0b])
            P.pool(lambda e, nR1=nR1, R1=R1: e.tensor_scalar(out=nR1[:], in0=R1[:], scalar1=-1.0, scalar2=None, op0=ALU.mult), r=[R1b], w=[nR1b])
            yield
            for d in range(2):
                cm = cfg[d]['cum']
                sl = slice(d * 4, (d + 1) * 4)
                P.pe(lambda e, cm=cm, sl=sl, R0=R0: e.matmul(psDv[:, sl, :], lhsT=cm, rhs=R0[:, sl, :], start=True, stop=False), r=[mkb, R0b], w=[psDb])
                P.pe(lambda e, sl=sl, nR1=nR1: e.matmul(psDv[:, sl, :], lhsT=ones64, rhs=nR1[:, sl, :], start=False, stop=False), r=[onesb, nR1b], w=[psDb])
                P.pe(lambda e, sl=sl, d=d: e.matmul(psDv[:, sl, :], lhsT=id64, rhs=mk[:, cfg[d]['m_strict'], :].unsqueeze(1).to_broadcast([64, 4, 64]), start=False, stop=True),
                     r=[idfb, mkb], w=[psDb])
                P.pe(lambda e, cm=cm, sl=sl, nR0=nR0: e.matmul(psTv[:, sl, :], lhsT=cm, rhs=nR0[:, sl, :], start=True, stop=False), r=[mkb, nR0b], w=[psTb])
                P.pe(lambda e, sl=sl, R1=R1: e.matmul(psTv[:, sl, :], lhsT=ones64, rhs=R1[:, sl, :], start=False, stop=False), r=[onesb, R1b], w=[psTb])
                P.pe(lambda e, sl=sl, d=d: e.matmul(psTv[:, sl, :], lhsT=id64, rhs=mk[:, cfg[d]['t_incl'], :].unsqueeze(1).to_broadcast([64, 4, 64]), start=False, stop=True),
                     r=[idfb, mkb], w=[psTb])
            yield
            dec, decb, _ = SL['dcr'].next()
            decT, decTb, _ = SL['dtr'].next()
            P.act(lambda e, dec=dec: e.activation(out=dec[:].rearrange("p a x -> p (a x)"), in_=psD[0:64, :], func=AF.Exp), r=[psDb], w=[decb])
            P.act(lambda e, decT=decT: e.activation(out=decT[:].rearrange("p a x -> p (a x)"), in_=psT[0:64, :], func=AF.Exp), r=[psTb], w=[decTb])
            psG, psGb = P.ps()
            for d in range(2):
                P.pe(lambda e, d=d: e.matmul(psG[0:64, d * 4:(d + 1) * 4], lhsT=cfg[d]['cum'], rhs=gb[:, d * 4:(d + 1) * 4], start=True, stop=True), r=[mkb, gbb], w=[psGb])
            P.pe(lambda e: e.matmul(psG[0:64, 8:16], lhsT=ones64, rhs=gb[:, 0:8], start=True, stop=True), r=[onesb, gbb], w=[psGb])
            s_, sb2, _ = SL['sc'].next()
            of, ofb, ofs = SL['outf'].next()
            ofv = of[:].rearrange("p d (k h) -> p d k h", h=4)
            P.act(lambda e, s_=s_, psG=psG: e.copy(out=s_[:, 0:16], in_=psG[0:64, 0:16]), r=[psGb], w=[sb2])
            P.dve(lambda e, s_=s_: e.tensor_tensor(out=s_[:, 16:24], in0=s_[:, 8:16], in1=s_[:, 0:8], op=ALU.subtract), r=[sb2], w=[sb2])
            P.act(lambda e, s_=s_: e.activation(out=s_[:, 0:24], in_=s_[:, 0:24], func=AF.Exp), r=[sb2], w=[sb2])
            s3 = s_[:, 0:24].rearrange("p (k d h) -> p k d h", d=2, h=4)
            beta = gb[:, 8:16].rearrange("p (d h) -> p d h", h=4)
            P.dve(lambda e, ofv=ofv, s3=s3, beta=beta: e.tensor_tensor(out=ofv[:, :, 0, :], in0=s3[:, 0, :, :], in1=beta, op=ALU.mult), r=[sb2, gbb], w=[ofb])
            P.pool(lambda e, ofv=ofv, s3=s3: e.tensor_copy(out=ofv[:, :, 1, :], in_=s3[:, 0, :, :]), r=[sb2], w=[ofb])
            P.pool(lambda e, ofv=ofv, s3=s3: e.tensor_copy(out=ofv[:, :, 2, :], in_=s3[:, 1, :, :]), r=[sb2], w=[ofb])
            yield
            ob, obb, obs = SL['outb'].next()
            obv = ob[:].rearrange("p d (k h x) -> p d k h x", k=4, h=4)
            ktok = tk[:, 0:256].rearrange("p (h x) -> p h x", x=64)
            vtok = tk[:, 256:512].rearrange("p (h x) -> p h x", x=64)
            for d in range(2):
                P.dve(lambda e, d=d, obv=obv, s_=s_, ktok=ktok: e.tensor_tensor(out=obv[:, d, 2, :, :], in0=ktok, in1=bc(s_[:, 16 + d * 4:20 + d * 4], [64, 4, 64]), op=ALU.mult),
                      r=[tkb, sb2], w=[obb])
                P.pool(lambda e, d=d, obv=obv, vtok=vtok: e.tensor_tensor(out=obv[:, d, 3, :, :], in0=vtok, in1=bc(gb[:, 8 + d * 4:12 + d * 4], [64, 4, 64]), op=ALU.mult),
                       r=[tkb, gbb], w=[obb])
                P.dve(lambda e, d=d, obv=obv, decT=decT: e.tensor_tensor(out=obv[:, d, 1, :, :], in0=kq[:, :, 0:64], in1=decT[:, d * 4:(d + 1) * 4, :], op=ALU.mult),
                      r=[kqb, decTb], w=[obb])
            N, Nb, _ = SL['nr_'].next()
            tm, tmb, _ = SL['tmpf'].next()
            for d in range(2):
                P.dve(lambda e, d=d, tm=tm, dec=dec: e.tensor_tensor(out=tm[:, d * 4:(d + 1) * 4, :], in0=kq[:, :, 64:128], in1=dec[:, d * 4:(d + 1) * 4, :], op=ALU.mult),
                      r=[kqb, decb], w=[tmb])
            P.dve(lambda e, N=N, tm=tm: e.scalar_tensor_tensor(out=N[:], in0=tm[:], scalar=-1.0, in1=bc(gb[:, 8:16], [64, 8, 64]), op0=ALU.mult, op1=ALU.mult),
                  r=[tmb, gbb], w=[Nb])
            yield
            psN, psNb = P.ps()
            psNv = psN[0:64, 0:256].bitcast(BF16).rearrange("p (a x) -> p a x", x=64)
            for a in range(8):
                P.pe(lambda e, a=a, N=N: e.transpose(out=psNv[:, a, :], in_=N[:, a, :], identity=id64b), r=[Nb, idbb], w=[psNb])
            yield
            NT, NTb, _ = SL['ntr'].next()
            P.act(lambda e, NT=NT: e.copy(out=NT[:], in_=psNv), r=[psNb], w=[NTb])
            TT, TTb, _ = SL['ttr'].next()
            P.pool(lambda e, TT=TT, NT=NT: e.tensor_tensor(out=TT[:], in0=NT[:], in1=id64b.unsqueeze(1).to_broadcast([64, 8, 64]), op=ALU.add), r=[NTb, idbb], w=[TTb])
            yield
            Pk, Pkb, PkT, PkTb = N, Nb, NT, NTb
            for lev in range(5):
                ps1, ps1b = P.ps()
                ps1v = ps1[0:64, :].rearrange("p (a x) -> p a x", x=64)
                for a in range(8):
                    P.pe(lambda e, a=a, PkT=PkT, Pk=Pk, ps1v=ps1v: e.matmul(ps1v[:, a, :], lhsT=PkT[:, a, :], rhs=Pk[:, a, :], start=True, stop=True), r=[PkTb, Pkb], w=[ps1b])
                if lev < 4:
                    ps2, ps2b = P.ps()
                    ps2v = ps2[0:64, :].rearrange("p (a x) -> p a x", x=64)
                    for a in range(8):
                        P.pe(lambda e, a=a, PkT=PkT, Pk=Pk, ps2v=ps2v: e.matmul(ps2v[:, a, :], lhsT=Pk[:, a, :], rhs=PkT[:, a, :], start=True, stop=True), r=[PkTb, Pkb], w=[ps2b])
                yield
                Pn, Pnb, _ = SL['p2r'].next()
                P.act(lambda e, Pn=Pn, ps1=ps1: e.copy(out=Pn[:].rearrange("p a x -> p (a x)"), in_=ps1[0:64, :]), r=[ps1b], w=[Pnb])
                if lev < 4:
                    PnT, PnTb, _ = SL['p2tr'].next()
                    P.dve(lambda e, PnT=PnT, ps2=ps2: e.tensor_copy(out=PnT[:].rearrange("p a x -> p (a x)"), in_=ps2[0:64, :]), r=[ps2b], w=[PnTb])
                yield
                ps3, ps3b = P.ps()
                ps3v = ps3[0:64, :].rearrange("p (a x) -> p a x", x=64)
                for a in range(8):
                    P.pe(lambda e, a=a, Pn=Pn, TT=TT, ps3v=ps3v: e.matmul(ps3v[:, a, :], lhsT=Pn[:, a, :], rhs=TT[:, a, :], start=True, stop=True), r=[Pnb, TTb], w=[ps3b])
                yield
                TT2, TT2b, _ = SL['ttr'].next()
                P.dve(lambda e, TT2=TT2, TT=TT, ps3=ps3: e.tensor_tensor(out=TT2[:].rearrange("p a x -> p (a x)"), in0=ps3[0:64, :], in1=TT[:].rearrange("p a x -> p (a x)"), op=ALU.add),
                      r=[ps3b, TTb], w=[TT2b])
                TT, TTb = TT2, TT2b
                Pk, Pkb = Pn, Pnb
                if lev < 4:
                    PkT, PkTb = PnT, PnTb
                yield
            for d in range(2):
                P.pool(lambda e, d=d, obv=obv, TT=TT: e.tensor_copy(out=obv[:, d, 0, :, :], in_=TT[:, d * 4:(d + 1) * 4, :]), r=[TTb], w=[obb])
            P.dma(PREPB[:, c].rearrange("d p x -> p d x"), ob[:], r=[obb], w=[P.B(('PB', c))], sem=obs)
            P.dma(PREPF[:, c].rearrange("d p x -> p d x"), of[:], r=[ofb], w=[P.B(('PF', c))], sem=ofs)
        def run_interleaved(gens_iter, width):
            active = []
            it = iter(gens_iter)
            done = False
            while True:
                while len(active) < width and not done:
                    try:
                        active.append(next(it))
                    except StopIteration:
                        done = True
                if not active:
                    break
                for g in list(active):
                    try:
                        next(g)
                    except StopIteration:
                        active.remove(g)
        run_interleaved((_prep(c) for c in range(NCH)), KP)
        P.release()

        P.mark()
        order = [list(range(NCH)), [3, 2, 1, 0] + list(range(NCH - 1, 3, -1))]
        DNFv = DNF.rearrange("i (h p) t -> p (i h) t", h=2)

        def mk_dir(d):
            return dict(S=P.sb([64, 4, 64], F32, f"S{d}"), Sb=Buf(), Sh=P.sb([64, 4, 64], BF16, f"Sh{d}"), Shb=Buf(),
                        pbr=Ring(P, [64, 1024], BF16, 3, f"pb{d}"), pfr=Ring(P, [64, 12], F32, 3, f"pf{d}"), fqr=Ring(P, [64, 8, 64], BF16, 3, f"fq2{d}"),
                        xr=Ring(P, [64, 4, 64], BF16, 2, f"X{d}", dma=False), vnr=Ring(P, [64, 4, 64], BF16, 2, f"vn{d}", dma=False),
                        t1r=Ring(P, [64, 4, 64], F32, 2, f"st1{d}", dma=False), osr=Ring(P, [64, 4, 64], F32, 3, f"os{d}"))
        dirs = [mk_dir(d) for d in range(2)]

        def v4(ps):
            return ps[0:64, 0:256].rearrange("p (a x) -> p a x", x=64)

        def _scan_dir(d):
            D = dirs[d]
            S, Sb_, Sh, Shb = D['S'], D['Sb'], D['Sh'], D['Shb']
            P.pool(lambda e: e.memset(S[:], 0.0), w=[Sb_])
            P.pool(lambda e: e.memset(Sh[:], 0.0), w=[Shb])
            for s_i in range(NCH):
                c = order[d][s_i]
                pb_, pbb_, pbs = D['pbr'].next()
                pf, pfb, pfs = D['pfr'].next()
                fq, fqb, fqs = D['fqr'].next()
                P.dma(pb_[:], PREPB[d, c], r=[P.B(('PB', c))], w=[pbb_], sem=pbs)
                P.dma(pf[:], PREPF[d, c], r=[P.B(('PF', c))], w=[pfb], sem=pfs)
                P.dma(fq[:], DNFv[:, :, c * 64:(c + 1) * 64], r=[P.B(('DNF', i)) for i in range(4)], w=[fqb], sem=fqs)
                pbv = pb_[:].rearrange("p (k h x) -> p k h x", k=4, h=4)
                pfv = pf[:].rearrange("p (k h) -> p k h", h=4)
                psK, psKb = P.ps()
                psQ, psQb = P.ps()
                psKv, psQv = v4(psK), v4(psQ)
                for hh in range(4):
                    P.pe(lambda e, hh=hh, fq=fq, psKv=psKv: e.matmul(psKv[:, hh, :], lhsT=fq[:, 4 + hh, :], rhs=Sh[:, hh, :], start=True, stop=True), r=[fqb, Shb], w=[psKb])
                for hh in range(4):
                    P.pe(lambda e, hh=hh, fq=fq, psQv=psQv: e.matmul(psQv[:, hh, :], lhsT=fq[:, hh, :], rhs=Sh[:, hh, :], start=True, stop=True), r=[fqb, Shb], w=[psQb])
                yield
                t1, t1b, _ = D['t1r'].next()
                X, Xb, _ = D['xr'].next()
                os_, osb, oss = D['osr'].next()
                P.dve(lambda e, t1=t1, psKv=psKv, pfv=pfv: e.tensor_tensor(out=t1[:], in0=psKv, in1=bc(pfv[:, 0, :], [64, 4, 64]), op=ALU.mult), r=[psKb, pfb], w=[t1b])
                P.dve(lambda e, t1=t1, X=X, pbv=pbv: e.tensor_tensor(out=X[:], in0=pbv[:, 3, :, :], in1=t1[:], op=ALU.subtract), r=[t1b, pbb_], w=[Xb])
                P.dve(lambda e, os_=os_, psQv=psQv, pfv=pfv: e.tensor_tensor(out=os_[:], in0=psQv, in1=bc(pfv[:, 1, :], [64, 4, 64]), op=ALU.mult), r=[psQb, pfb], w=[osb])
                yield
                psV, psVb = P.ps()
                psVv = v4(psV)
                for hh in range(4):
                    P.pe(lambda e, hh=hh, pbv=pbv, X=X, psVv=psVv: e.matmul(psVv[:, hh, :], lhsT=pbv[:, 0, hh, :], rhs=X[:, hh, :], start=True, stop=True), r=[pbb_, Xb], w=[psVb])
                yield
                vn, vnb, _ = D['vnr'].next()
                P.act(lambda e, vn=vn, psVv=psVv: e.copy(out=vn[:], in_=psVv), r=[psVb], w=[vnb])
                yield
                psS, psSb = P.ps()
                psO, psOb = P.ps()
                psSv, psOv = v4(psS), v4(psO)
                for hh in range(4):
                    P.pe(lambda e, hh=hh, pbv=pbv, vn=vn, psSv=psSv: e.matmul(psSv[:, hh, :], lhsT=pbv[:, 2, hh, :], rhs=vn[:, hh, :], start=True, stop=True), r=[pbb_, vnb], w=[psSb])
                for hh in range(4):
                    P.pe(lambda e, hh=hh, pbv=pbv, vn=vn, psOv=psOv: e.matmul(psOv[:, hh, :], lhsT=pbv[:, 1, hh, :], rhs=vn[:, hh, :], start=True, stop=True), r=[pbb_, vnb], w=[psOb])
                yield
                P.dve(lambda e, pfv=pfv: e.tensor_tensor(out=S[:], in0=S[:], in1=bc(pfv[:, 2, :], [64, 4, 64]), op=ALU.mult), r=[Sb_, pfb], w=[Sb_])
                P.dve(lambda e, psSv=psSv: e.tensor_tensor(out=S[:], in0=S[:], in1=psSv, op=ALU.add), r=[Sb_, psSb], w=[Sb_])
                P.act(lambda e: e.copy(out=Sh[:], in_=S[:]), r=[Sb_], w=[Shb])
                P.dve(lambda e, os_=os_, psOv=psOv: e.tensor_tensor(out=os_[:], in0=os_[:], in1=psOv, op=ALU.add), r=[osb, psOb], w=[osb])
                if not (c < 4 and not ctx_out):
                    P.dma(OSC[d, c * 64:(c + 1) * 64, :], os_[:].rearrange("p a x -> p (a x)"), r=[osb], w=[P.B(('O', d, c))], sem=oss)
                yield
        run_interleaved((_scan_dir(d) for d in range(2)), 2)
        P.release()

        P.mark()
        o0r = Ring(P, [128, 2, 256], F32, 2, "o0")
        zr = Ring(P, [128, 256], F32, 2, "zr")
        sqr = Ring(P, [128, 256], F32, 2, "sqo")
        ssr = Ring(P, [128, 4], F32, 2, "sso")
        yr = Ring(P, [128, 256], BF16, 2, "yo")
        ngt = P.sb([128, 256], F32, "ngt"); ngtb = Buf(); ngts = P.new_dsem()
        e64 = P.sb([128, 1], F32, "e64"); e64b = Buf()
        P.pool(lambda e: e.memset(e64[:], EPS), w=[e64b])
        P.dma(ngt[:], dnng[l].partition_broadcast(128), w=[ngtb], sem=ngts)
        tst = Ring(P, [128, 2, 128], BF16, 2, "tst2")
        def _og(tt):
            if tt < 2 and not ctx_out:
                return
            o0, o0b, o0s = o0r.next()
            z, zb, zs = zr.next()
            P.dma(o0[:], OSC[:, tt * 128:(tt + 1) * 128, :].rearrange("d p x -> p d x"), r=[P.B(('O', d, 2 * tt + k)) for d in range(2) for k in range(2)], w=[o0b], sem=o0s)
            P.dma(z[:], ZAB[tt * 128:(tt + 1) * 128, 0:256], r=[P.B(('Z', tt))], w=[zb], sem=zs)
            P.pool(lambda e, o0=o0: e.tensor_tensor(out=o0[:, 0, :], in0=o0[:, 0, :], in1=o0[:, 1, :], op=ALU.add), r=[o0b], w=[o0b])
            sq, sqb, _ = sqr.next()
            P.act(lambda e, sq=sq, o0=o0: e.activation(out=sq[:], in_=o0[:, 0, :], func=AF.Square), r=[o0b], w=[sqb])
            ss_, ssb, _ = ssr.next()
            P.dve(lambda e, ss_=ss_, sq=sq: e.tensor_reduce(out=ss_[:], in_=sq[:].rearrange("p (h x) -> p h x", x=64), axis=AX.X, op=ALU.add), r=[sqb], w=[ssb])
            P.act(lambda e, ss_=ss_: e.activation(out=ss_[:], in_=ss_[:], func=AF.Sqrt, bias=e64[:], scale=1.0 / 64), r=[ssb, e64b], w=[ssb])
            P.dve(lambda e, ss_=ss_: e.reciprocal(out=ss_[:], in_=ss_[:]), r=[ssb], w=[ssb])
            P.dve(lambda e, sq=sq, o0=o0, ss_=ss_: e.tensor_tensor(out=sq[:].rearrange("p (h x) -> p h x", x=64), in0=o0[:, 0, :].rearrange("p (h x) -> p h x", x=64), in1=bc(ss_[:, 0:4], [128, 4, 64]), op=ALU.mult),
                  r=[o0b, ssb], w=[sqb])
            P.pool(lambda e, sq=sq: e.tensor_tensor(out=sq[:], in0=sq[:], in1=ngt[:], op=ALU.mult), r=[sqb, ngtb], w=[sqb])
            y, yb, _ = yr.next()
            P.dve(lambda e, y=y, sq=sq, z=z: e.tensor_tensor(out=y[:], in0=sq[:], in1=z[:], op=ALU.mult), r=[sqb, zb], w=[yb])
            st, sb_, ss2 = tst.next()
            for k in range(2):
                ps, pb = P.ps()
                psv = ps[:, 0:64].bitcast(BF16)
                P.pe(lambda e, psv=psv, y=y, k=k: e.transpose(out=psv, in_=y[:, k * 128:(k + 1) * 128], identity=idb[:]), r=[yb, idbb], w=[pb])
                P.act(lambda e, psv=psv, st=st, k=k: e.copy(out=st[:, k, :], in_=psv), r=[pb], w=[sb_])
            P.dma(DNY[:, :, tt * 128:(tt + 1) * 128].rearrange("k p t -> p k t"), st[:], r=[sb_], w=[P.B(('DNY', tt))], sem=ss2)
        for tt in range(34):
            _og(tt)
        P.release()

    def phase_merge(l):
        ctx_out = l == 0
        P.mark()
        wb = P.sb([128, 8, 1024], BF16, "wbr"); wbb = Buf(); wbs = P.new_dsem()
        wo = P.sb([128, 8, 1024], BF16, "wo"); wob = Buf(); wos = P.new_dsem()
        P.dma(wb[:], w_br[l].rearrange("c p n -> p c n"), w=[wbb], sem=wbs, q='pool')
        P.dma(wo[:], w_out[l].rearrange("c p n -> p c n"), w=[wob], sem=wos, q='pool')
        inr = Ring(P, [128, 8, 512], BF16, 2, "min")
        gr = Ring(P, [128, 24, 512], BF16, 2, "gin")
        hr = Ring(P, [128, 8, 512], F32, 2, "hin")
        mixr = Ring(P, [128, 8, 512], BF16, 2, "mix")
        tr_ = Ring(P, [128, 512], F32, 6, "mt")
        src = hsrc_of(l)
        own = SPLIT and l == 1

        def _mb(t0, n):
            if t0 == 0 and not ctx_out:
                return
            s_ = 1 if t0 == 0 else 0
            xin, xb, xs = inr.next()
            g, gb_, gs = gr.next()
            h, hb_, hs = hr.next()
            P.dma(xin[:, 0:4, :n], ATT[:, :, t0:t0 + n].rearrange("c p t -> p c t"), r=[P.B(('ATT', hh, t0)) for hh in range(4)], w=[xb], sem=xs)
            if own:
                cands = (t0, t0 + 2048)
                P.dmad(xin[:, 4:6, :n], lambda off: DNY[:, :, bass.ds(t0 + off, n)].rearrange("c p t -> p c t"),
                       r=[P.B(('DNY', tt)) for tc in cands for tt in range(tc // 128, (tc + n) // 128)], w=[xb], sem=xs)
                P.dmad(xin[:, 6:8, :n], lambda off: POOLT[:, :, bass.ds(t0 + off, n)].rearrange("c p t -> p c t"), r=[P.B(('POOL', i)) for i in range(2)], w=[xb], sem=xs)
                P.dmad(g[:, :, :n], lambda off: GT[:, :, bass.ds(t0 + off, n)].rearrange("c p t -> p c t"), r=[P.B(('G', j, tc)) for j in range(24) for tc in cands], w=[gb_], sem=gs)
                P.dmad(h[:, :, :n], lambda off: src[:, :, bass.ds(t0 + off, n)].rearrange("c p t -> p c t"), r=[P.B(('H', tc)) for tc in cands], w=[hb_], sem=hs)
            else:
                P.dma(xin[:, 4:6, :n], DNY[:, :, t0:t0 + n].rearrange("c p t -> p c t"), r=[P.B(('DNY', tt)) for tt in range(t0 // 128, (t0 + n) // 128)], w=[xb], sem=xs)
                P.dma(xin[:, 6:8, :n], POOLT[:, :, t0:t0 + n].rearrange("c p t -> p c t"), r=[P.B(('POOL', i)) for i in range(2)], w=[xb], sem=xs)
                P.dma(g[:, :, :n], GT[:, :, t0:t0 + n].rearrange("c p t -> p c t"), r=[P.B(('G', j, t0)) for j in range(24)], w=[gb_], sem=gs)
                P.dma(h[:, :, :n], src[:, :, t0:t0 + n].rearrange("c p t -> p c t"), r=[P.B(('H', t0))], w=[hb_], sem=hs)
            mix, mixb, _ = mixr.next()
            for c in range(8):
                cs = slice(c * 128, (c + 1) * 128)
                psa, psab = P.ps()
                for k in range(4):
                    P.pe(lambda e, k=k, cs=cs, psa=psa, xin=xin: e.matmul(psa[:, :n], lhsT=wb[:, k, cs], rhs=xin[:, k, :n], start=(k == 0), stop=(k == 3)), r=[wbb, xb], w=[psab])
                psd, psdb = P.ps()
                for k in range(2):
                    P.pe(lambda e, k=k, cs=cs, psd=psd, xin=xin: e.matmul(psd[:, :n], lhsT=wb[:, 4 + k, cs], rhs=xin[:, 4 + k, :n], start=(k == 0), stop=(k == 1)), r=[wbb, xb], w=[psdb])
                psp, pspb = P.ps()
                for k in range(2):
                    P.pe(lambda e, k=k, cs=cs, psp=psp, xin=xin: e.matmul(psp[:, :n], lhsT=wb[:, 6 + k, cs], rhs=xin[:, 6 + k, :n], start=(k == 0), stop=(k == 1)), r=[wbb, xb], w=[pspb])
                t1, t1b, _ = tr_.next()
                t2, t2b, _ = tr_.next()
                t3, t3b, _ = tr_.next()
                P.dve(lambda e, t1=t1, psa=psa, g=g, c=c: e.tensor_tensor(out=t1[:, :n], in0=psa[:, :n], in1=g[:, c, :n], op=ALU.mult), r=[psab, gb_], w=[t1b])
                P.dve(lambda e, t2=t2, psd=psd, g=g, c=c: e.tensor_tensor(out=t2[:, :n], in0=psd[:, :n], in1=g[:, 8 + c, :n], op=ALU.mult), r=[psdb, gb_], w=[t2b])
                P.dve(lambda e, t3=t3, psp=psp, g=g, c=c: e.tensor_tensor(out=t3[:, :n], in0=psp[:, :n], in1=g[:, 16 + c, :n], op=ALU.mult), r=[pspb, gb_], w=[t3b])
                P.pool(lambda e, t1=t1, t2=t2: e.tensor_tensor(out=t1[:, :n], in0=t1[:, :n], in1=t2[:, :n], op=ALU.add), r=[t1b, t2b], w=[t1b])
                P.pool(lambda e, t1=t1, t3=t3, mix=mix, c=c: e.tensor_tensor(out=mix[:, c, :n], in0=t1[:, :n], in1=t3[:, :n], op=ALU.add), r=[t1b, t3b], w=[mixb])
            for c in range(8):
                cs = slice(c * 128, (c + 1) * 128)
                psy, psyb = P.ps()
                for k in range(8):
                    P.pe(lambda e, k=k, cs=cs, psy=psy, mix=mix: e.matmul(psy[:, :n], lhsT=wo[:, k, cs], rhs=mix[:, k, :n], start=(k == 0), stop=(k == 7)), r=[wob, mixb], w=[psyb])
                P.dve(lambda e, c=c, psy=psy, h=h, s_=s_: e.scalar_tensor_tensor(out=h[:, c, :n], in0=psy[:, :n], scalar=mod[:, s_, 16 + c:17 + c], in1=h[:, c, :n], op0=ALU.mult, op1=ALU.add),
                      r=[psyb, modb, hb_], w=[hb_])
            P.dma(H[:, :, t0:t0 + n].rearrange("c p t -> p c t"), h[:, :, :n], r=[hb_], w=[P.B(('H', t0))], sem=hs)
        for (t0, n) in ([(256 + 512 * j, 512) for j in range(4)] if own else BLKS):
            _mb(t0, n)
        P.release()

    def phase_ffn(l, final):
        moe = (l == 1)
        ctx_out = l == 0
        NF = 28 if moe else 22
        P.mark()
        SBK = 1024
        hb_t = P.sb([128, 8, SBK], F32, "hsb"); hbb = Buf(); hbs = P.new_dsem()
        u2 = P.sb([128, 8, SBK], BF16, "u2"); u2b = Buf()
        hid = P.sb([128, NF, SBK], BF16, "hid"); hidb = Buf()
        rs = ln_rings()
        w13 = Ring(P, [128, 2, 8, 512], BF16, 2, "w13")
        w2r = Ring(P, [128, 4, 1024], BF16, 2, "w2")
        sr = Ring(P, [128, 512], F32, 4, "sil", dma=False)
        if moe:
            rwt = P.sb([128, 8, 8], F32, "rwt"); rwb = Buf(); rws = P.new_dsem()
            P.dma(rwt[:], rw, w=[rwb], sem=rws)
            selt = P.sb([8, 8, 128], F32, "selt"); selb = Buf()
            P.dma(selt[:], selc, w=[selb], sem=rws)
            lg = P.sb([128, 8, 8], F32, "lg"); lgb = Buf()
            cmb = P.sb([128, 8, 8], F32, "cmb"); cmbb = Buf()
            combT = P.sb([8, SBK], F32, "combT"); combTb = Buf()
            cb = P.sb([128, SBK], F32, "cb"); cbb = Buf()
            sm = Ring(P, [128, 8], F32, 4, "sm")
            sm1 = Ring(P, [128, 1], F32, 6, "sm1")
        sblocks = ([(0, 256)] if (ctx_out and not final) else []) + [(256 + SBK * i, SBK) for i in range((TOUT if (SPLIT and l == 1) else TL) // SBK)]
        if final:
            pass
        for (T0, NS) in sblocks:
            s_ = 1 if T0 == 0 else 0
            P.dma(hb_t[:, :, :NS], H[:, :, T0:T0 + NS].rearrange("c p t -> p c t"), r=[P.B(('H', t)) for t in range(T0, T0 + NS, 512)], w=[hbb], sem=hbs)
            nb = (NS + 511) // 512
            for bi in range(nb):
                n = min(512, NS - bi * 512)
                o = bi * 512
                if moe:
                    psRs = [P.ps() for _ in range(4)]

                    def hook(c, tmp, tb, tsem, psRs=psRs, n=n):
                        for tt in range(n // 128):
                            psR, psRb = psRs[tt]
                            P.pe(lambda e, tt=tt, c=c, tmp=tmp, psR=psR: e.matmul(psR[:, 0:8], lhsT=tmp[:, tt * 128:(tt + 1) * 128], rhs=rwt[:, c, :],
                                                                                  start=(c == 0), stop=(c == 7)), r=[tb, rwb], w=[psRb])
                else:
                    hook = None
                ln_block(hb_t[:, :, o:o + n], hbb, n, lambda c, s_=s_: gain[:, 1, s_, c:c + 1], lambda c, s_=s_: mod[:, s_, 24 + c:25 + c],
                         u2, u2b, o, rs, f32hook=hook)
                if moe:
                    for tt in range(4):
                        psR, psRb = psRs[tt]
                        P.act(lambda e, psR=psR, bi=bi, tt=tt: e.copy(out=lg[:, bi * 4 + tt, :], in_=psR[:, 0:8]), r=[psRb], w=[lgb])
            if moe:
                for tt in range(NS // 128):
                    m1, m1b, _ = sm1.next()
                    m2, m2b, _ = sm1.next()
                    dn_, dnb, _ = sm1.next()
                    l2_, l2b_, _ = sm.next()
                    w_, wb2, _ = sm.next()
                    lgt = lg[:, tt, :]
                    P.dve(lambda e, m1=m1, lgt=lgt: e.tensor_reduce(out=m1[:], in_=lgt, axis=AX.X, op=ALU.max), r=[lgb], w=[m1b])
                    P.dve(lambda e, l2_=l2_, lgt=lgt, m1=m1: e.tensor_scalar(out=l2_[:], in0=lgt, scalar1=m1[:, 0:1], scalar2=-1e30, op0=ALU.is_equal, op1=ALU.mult), r=[lgb, m1b], w=[l2b_])
                    P.dve(lambda e, l2_=l2_, lgt=lgt: e.tensor_tensor(out=l2_[:], in0=l2_[:], in1=lgt, op=ALU.add), r=[l2b_, lgb], w=[l2b_])
                    P.dve(lambda e, m2=m2, l2_=l2_: e.tensor_reduce(out=m2[:], in_=l2_[:], axis=AX.X, op=ALU.max), r=[l2b_], w=[m2b])
                    P.dve(lambda e, l2_=l2_, lgt=lgt, m2=m2: e.tensor_scalar(out=l2_[:], in0=lgt, scalar1=m2[:, 0:1], scalar2=None, op0=ALU.is_ge), r=[lgb, m2b, l2b_], w=[l2b_])
                    P.dve(lambda e, w_=w_, lgt=lgt, m1=m1: e.tensor_scalar(out=w_[:], in0=lgt, scalar1=m1[:, 0:1], scalar2=None, op0=ALU.subtract), r=[lgb, m1b], w=[wb2])
                    P.act(lambda e, w_=w_: e.activation(out=w_[:], in_=w_[:], func=AF.Exp), r=[wb2], w=[wb2])
                    P.dve(lambda e, w_=w_, l2_=l2_: e.tensor_tensor(out=w_[:], in0=w_[:], in1=l2_[:], op=ALU.mult), r=[wb2, l2b_], w=[wb2])
                    P.dve(lambda e, dn_=dn_, w_=w_: e.tensor_reduce(out=dn_[:], in_=w_[:], axis=AX.X, op=ALU.add), r=[wb2], w=[dnb])
                    P.dve(lambda e, dn_=dn_: e.reciprocal(out=dn_[:], in_=dn_[:]), r=[dnb], w=[dnb])
                    P.dve(lambda e, tt=tt, w_=w_, dn_=dn_: e.tensor_scalar(out=cmb[:, tt, :], in0=w_[:], scalar1=dn_[:, 0:1], scalar2=None, op0=ALU.mult), r=[wb2, dnb], w=[cmbb])
                for half in range(NS // 512):
                    psC, psCb = P.ps()
                    for tq in range(4):
                        tt = half * 4 + tq
                        P.pe(lambda e, tt=tt, tq=tq, psC=psC: e.transpose(out=psC[0:8, tq * 128:(tq + 1) * 128], in_=cmb[:, tt, :], identity=idf[:]), r=[cmbb, idfb], w=[psCb])
                    P.act(lambda e, half=half, psC=psC: e.copy(out=combT[:, half * 512:(half + 1) * 512], in_=psC[0:8, :]), r=[psCb], w=[combTb])
            for ex in range(8 if moe else 1):
                if moe:
                    W1 = mw1[ex]; W3 = mw3[ex]; W2 = mw2[ex]
                    for half in range(NS // 512):
                        psB, psBb = P.ps()
                        P.pe(lambda e, ex=ex, half=half, psB=psB: e.matmul(psB[:, :], lhsT=selt[:, ex, :], rhs=combT[:, half * 512:(half + 1) * 512], start=True, stop=True), r=[selb, combTb], w=[psBb])
                        P.act(lambda e, half=half, psB=psB: e.copy(out=cb[:, half * 512:(half + 1) * 512], in_=psB[:, :]), r=[psBb], w=[cbb])
                else:
                    W1 = fw1; W3 = fw3; W2 = fw2
                HC = NF * 128
                for c0 in range(0, HC, 512):
                    gw = min(512, HC - c0)
                    wt, wtb, wts = w13.next()
                    P.dma(wt[:, 0, :, :gw], W1[:, :, c0:c0 + gw].rearrange("c p n -> p c n"), w=[wtb], sem=wts, q='pool')
                    P.dma(wt[:, 1, :, :gw], W3[:, :, c0:c0 + gw].rearrange("c p n -> p c n"), w=[wtb], sem=wts, q='pool')
                    for j in range(gw // 128):
                        f = c0 // 128 + j
                        for bi in range(nb):
                            n = min(512, NS - bi * 512)
                            o = bi * 512
                            ps1, ps1b = P.ps()
                            for c in range(8):
                                P.pe(lambda e, c=c, j=j, ps1=ps1, wt=wt, o=o, n=n: e.matmul(ps1[:, :n], lhsT=wt[:, 0, c, j * 128:(j + 1) * 128], rhs=u2[:, c, o:o + n], start=(c == 0), stop=(c == 7)), r=[wtb, u2b], w=[ps1b])
                            ps3, ps3b = P.ps()
                            for c in range(8):
                                P.pe(lambda e, c=c, j=j, ps3=ps3, wt=wt, o=o, n=n: e.matmul(ps3[:, :n], lhsT=wt[:, 1, c, j * 128:(j + 1) * 128], rhs=u2[:, c, o:o + n], start=(c == 0), stop=(c == 7)), r=[wtb, u2b], w=[ps3b])
                            sl, slb, _ = sr.next()
                            P.act(lambda e, sl=sl, ps1=ps1, n=n: e.activation(out=sl[:, :n], in_=ps1[:, :n], func=AF.Silu), r=[ps1b], w=[slb])
                            if moe:
                                g3, g3b, _ = sr.next()
                                P.dve(lambda e, g3=g3, ps3=ps3, o=o, n=n: e.tensor_tensor(out=g3[:, :n], in0=ps3[:, :n], in1=cb[:, o:o + n], op=ALU.mult), r=[ps3b, cbb], w=[g3b])
                                P.dve(lambda e, sl=sl, g3=g3, f=f, o=o, n=n: e.tensor_tensor(out=hid[:, f, o:o + n], in0=sl[:, :n], in1=g3[:, :n], op=ALU.mult), r=[slb, g3b], w=[hidb])
                            else:
                                P.dve(lambda e, sl=sl, ps3=ps3, f=f, o=o, n=n: e.tensor_tensor(out=hid[:, f, o:o + n], in0=sl[:, :n], in1=ps3[:, :n], op=ALU.mult), r=[slb, ps3b], w=[hidb])
                for k0 in range(0, NF, 4):
                    kg = min(4, NF - k0)
                    w2, w2b, w2s = w2r.next()
                    P.dma(w2[:, :kg, :], W2[k0:k0 + kg].rearrange("k p n -> p k n"), w=[w2b], sem=w2s, q='pool')
                    for c in range(8):
                        for bi in range(nb):
                            n = min(512, NS - bi * 512)
                            o = bi * 512
                            pso, psob = P.ps()
                            for k in range(kg):
                                P.pe(lambda e, k=k, c=c, pso=pso, w2=w2, o=o, n=n, k0=k0, kg=kg: e.matmul(pso[:, :n], lhsT=w2[:, k, c * 128:(c + 1) * 128], rhs=hid[:, k0 + k, o:o + n], start=(k == 0), stop=(k == kg - 1)), r=[w2b, hidb], w=[psob])
                            P.dve(lambda e, c=c, pso=pso, o=o, n=n, s_=s_: e.scalar_tensor_tensor(out=hb_t[:, c, o:o + n], in0=pso[:, :n], scalar=mod[:, s_, 40 + c:41 + c], in1=hb_t[:, c, o:o + n], op0=ALU.mult, op1=ALU.add),
                                  r=[psob, modb, hbb], w=[hbb])
            if not final:
                for bi in range(nb):
                    n = min(512, NS - bi * 512)
                    P.dma(H[:, :, T0 + bi * 512:T0 + bi * 512 + n].rearrange("c p t -> p c t"), hb_t[:, :, bi * 512:bi * 512 + n], r=[hbb], w=[P.B(('H', T0 + bi * 512))], sem=hbs)
            else:
                for bi in range(nb):
                    o = bi * 512
                    pos = T0 - 256 + o

                    def hookf(c, tmp, tb, tsem, pos=pos):
                        P.dma(yT[c, :, pos:pos + 512], tmp[:, :], r=[tb], w=[P.B(('Y', pos, c))], sem=tsem)
                    ln_block(hb_t[:, :, o:o + 512], hbb, 512, lambda c: ng[:, 4, c:c + 1], None, None, None, 0, rs, f32hook=hookf)
        P.touch(hb_t, hbb)
        P.release()

    for l in range(n_layers):
        ada_layer(l)
        phase_proj(l)
        phase_attn(l)
        phase_dn(l)
        phase_merge(l)
        phase_ffn(l, final=(l == n_layers - 1))
    P.emit()
    return nc


def _rope_partner_cols():
    idx = np.zeros(512, np.int64)
    for h in range(4):
        for m in range(2):
            for d in range(64):
                half = d // 32
                dd = d % 32
                pd = dd + 16 if dd < 16 else dd - 16
                idx[h * 128 + m * 64 + d] = h * 128 + m * 64 + half * 32 + pd
    return idx


def _rope_tables():
    n_freq = 16
    inv = (10000.0 ** (-np.arange(n_freq, dtype=np.float32) / n_freq)).astype(np.float32)
    t = np.arange(TL)
    row = (t // 64).astype(np.float32)
    col = (t % 64).astype(np.float32)
    ar = row[:, None] * inv
    ac = col[:, None] * inv
    C = np.ones((128, T), np.float32)
    S = np.zeros((128, T), np.float32)
    for p in range(128):
        d = p % 64
        half = d // 32
        dd = d % 32
        f = dd % 16
        ang = (ar if half == 0 else ac)[:, f]
        C[p, TC:] = np.cos(ang)
        S[p, TC:] = (-np.sin(ang)) if dd < 16 else np.sin(ang)
    return C, S


def _pool_inv():
    out = np.zeros((2, 128, 4376), np.float32)
    wins = (2, 4, 8, 16)
    for g, w in enumerate(wins):
        for (L, off) in ((TC, 8), (TL, 272)):
            t = np.arange(L)
            lo = np.clip(t - w // 2, 0, L)
            hi = np.clip(t - w // 2 + w, 0, L)
            out[g // 2, (g % 2) * 64:(g % 2 + 1) * 64, off:off + L] = (1.0 / (hi - lo).astype(np.float32))[None, :]
    return out


def _masks():
    i = np.arange(64)
    m = np.zeros((64, 6, 64), np.float32)
    NEG = -30000.0
    m[:, 0, :] = (i[:, None] <= i[None, :])
    m[:, 1, :] = (i[:, None] >= i[None, :])
    r = i[:, None]; c = i[None, :]
    m[:, 2, :] = np.where(r < c, NEG, 0.0)
    m[:, 3, :] = np.where(r <= c, NEG, 0.0)
    m[:, 4, :] = np.where(r > c, NEG, 0.0)
    m[:, 5, :] = np.where(r >= c, NEG, 0.0)
    return m


def make_inputs(b, inp):
    f = np.float32
    A = np.ascontiguousarray
    d = {}
    full = np.concatenate([inp['ctx'][b], inp['x'][b]], axis=0)
    d['xT'] = A(full.T.reshape(8, 128, T))
    cin = np.stack([inp['c'][b], inp['c_ctx']], axis=-1)
    d['cin'] = A(cin.reshape(8, 128, 2).transpose(1, 0, 2))
    d['ada_w'] = inp['ada_w'].reshape(2, 8, 128, 6144)
    d['adab'] = A(inp['ada_b'].reshape(2, 48, 128).transpose(0, 2, 1))
    ngs = np.stack([inp['norm1_g'][0], inp['norm1_g'][1], inp['norm2_g'][0], inp['norm2_g'][1], inp['final_norm_g']])
    d['ngs'] = A(ngs.reshape(5, 8, 128).transpose(2, 0, 1))
    d['w_in'] = inp['w_in'].reshape(2, 8, 128, INW)
    pidx = _rope_partner_cols()
    cols = np.concatenate([pidx, 512 + pidx])
    d['w_inp'] = A(inp['w_in'][:, :, cols]).reshape(2, 8, 128, 1024)
    C, S = _rope_tables()
    d['ropeC'] = C
    d['ropeS'] = S
    d['lam_in'] = inp['attn_lambda'].reshape(2, 256)
    sg = np.zeros((128, 2), f)
    for l in range(2):
        li = 0.8 - 0.6 * math.exp(-0.3 * l)
        sg[:, l] = inp['attn_subln_g'][l] * np.float32(1 - li)
    d['sublng'] = sg
    d['convw'] = A(inp['dn_conv_w'].reshape(2, 5, 6, 128).transpose(0, 3, 2, 1))
    d['dnc'] = A(np.concatenate([inp['dn_dt_bias'].reshape(2, 8), inp['dn_a_log'].reshape(2, 8)], axis=1))
    d['dnng'] = A(np.tile(inp['dn_norm_g'], (1, 4)))
    d['pinv'] = _pool_inv()
    pw = np.zeros((2, 2, 128, 128), f)
    for l in range(2):
        for g in range(4):
            pw[l, g // 2, (g % 2) * 64:(g % 2 + 1) * 64, (g % 2) * 64:(g % 2 + 1) * 64] = inp['pool_w'][l, g]
    d['poolw'] = pw
    d['pools'] = A(inp['pool_scale'].reshape(2, 2, 128).transpose(2, 0, 1))
    d['w_br'] = inp['w_branch'].reshape(2, 8, 128, 1024)
    d['w_out'] = inp['w_out'].reshape(2, 8, 128, 1024)
    d['fw1'] = inp['ffn_w1'].reshape(8, 128, 2816)
    d['fw3'] = inp['ffn_w3'].reshape(8, 128, 2816)
    d['fw2'] = inp['ffn_w2'].reshape(22, 128, 1024)
    d['rw'] = A(inp['router_w'][0].reshape(8, 128, 8).transpose(1, 0, 2))
    d['mw1'] = inp['moe_w1'].reshape(8, 8, 128, 3584)
    d['mw3'] = inp['moe_w3'].reshape(8, 8, 128, 3584)
    d['mw2'] = inp['moe_w2'].reshape(8, 28, 128, 1024)
    sel = np.zeros((8, 8, 128), f)
    for e in range(8):
        sel[e, e, :] = 1.0
    d['selc'] = sel
    d['masks'] = _masks()
    d['identb'] = np.eye(128, dtype=f)
    return {k: np.ascontiguousarray(v, dtype=np.float32) for k, v in d.items()}


def kernel(**inputs):
    inp = {k: np.asarray(v, dtype=np.float32) for k, v in inputs.items()}
    nc = build()
    maps = [make_inputs(b, inp) for b in range(4)]
    in_maps = [maps[i % 4] for i in range(8)]
    res = run_bass_kernel_spmd(nc, in_maps, core_ids=list(range(8)))
    out = np.zeros((4, TL, 1024), np.float32)
    for i in range(8):
        b, half = i % 4, i // 4
        y = np.asarray(res.results[i]["yT"])
        if USE_SPLIT:
            out[b, half * 2048:(half + 1) * 2048] = y.reshape(1024, 2048).T
        elif half == 0:
            out[b] = y.reshape(1024, TL).T
    return out
```
